# Optimizing a Trainium2 kernel written in Bass

```python
import math
import jax
import jax.numpy as jnp
from jax import lax
import numpy as np

D_MODEL = 1024
BATCH = 16
SEQ = 2048
DEPTH = 2

GRID_W = 64
CTX_LEN = 256
N_EVEN = (DEPTH + 1) // 2
N_ODD = DEPTH // 2

A_WIDTH = D_MODEL // 2
DIFF_HEADS = 4
DIFF_DH = 64
DIFF_VDIM = 2 * DIFF_DH
DIFF_QK = DIFF_HEADS * 2 * DIFF_DH
B_WIDTH = DIFF_HEADS * DIFF_VDIM
EVEN_IN = 3 * A_WIDTH + 2 * DIFF_QK + B_WIDTH
EVEN_MIX = A_WIDTH + B_WIDTH

C_WIDTH = D_MODEL // 2
C_GROUPS = 4
C_GROUP_DIM = C_WIDTH // C_GROUPS
NA_HEADS = 8
NA_DH = 64
D_WIDTH = NA_HEADS * NA_DH
ODD_IN = C_WIDTH + 3 * D_WIDTH
ODD_MIX = C_WIDTH + D_WIDTH
NA_KR_MAX = 8
NA_KC = 16

D_FF = 2816
N_EXPERTS = 8
TOP_K = 2
D_FF_EXPERT = 3584

ROPE_THETA = 10000.0
LN_EPS = 1e-5
RMS_EPS = 1e-5
Q_BLOCK = 128
NEG_INF = -1e30
DEEPNORM_ALPHA = (2 * DEPTH) ** 0.25
DEEPNORM_BETA = (8 * DEPTH) ** -0.25

kernel_name = 'hybrid_diffusion_conv_diffattn_fnet_natten_moe'


def layer_norm(x, g, b):
    xf = x.astype(jnp.float32)
    mu = jnp.mean(xf, axis=-1, keepdims=True)
    var = jnp.mean(jnp.square(xf - mu), axis=-1, keepdims=True)
    return ((xf - mu) * lax.rsqrt(var + LN_EPS) * g + b).astype(x.dtype)


def adaln(cond, w_mod, b_mod):
    m = (jax.nn.silu(cond) @ w_mod + b_mod)[..., None, :]
    return jnp.split(m, 6, axis=-1)


def modulate(x, shift, scale):
    return x * (1 + scale) + shift


def short_conv3(u, w):
    up = jnp.pad(u, ((0, 0), (1, 1), (0, 0)))
    return up[:, :-2] * w[0] + up[:, 1:-1] * w[1] + up[:, 2:] * w[2]


def short_conv_mixer(pa, w_conv):
    bg, cg, val = jnp.split(pa, 3, axis=-1)
    return bg * short_conv3(cg * val, w_conv)


def axial_rope(n_tok, head_dim):
    t = jnp.arange(n_tok, dtype=jnp.int32)
    row = (t // GRID_W).astype(jnp.float32)
    col = (t % GRID_W).astype(jnp.float32)
    n_freq = head_dim // 4
    inv_freq = ROPE_THETA ** (-jnp.arange(n_freq, dtype=jnp.float32) / n_freq)
    ang = jnp.concatenate([row[:, None] * inv_freq, col[:, None] * inv_freq], axis=-1)
    return jnp.cos(ang), jnp.sin(ang)


def apply_rope(x, cos, sin):
    shape = (1, cos.shape[0]) + (1,) * (x.ndim - 3) + (cos.shape[1],)
    c = cos.reshape(shape)
    s = sin.reshape(shape)
    x1, x2 = jnp.split(x.astype(jnp.float32), 2, axis=-1)
    return jnp.concatenate([x1 * c - x2 * s, x1 * s + x2 * c], axis=-1).astype(x.dtype)


def diff_attend(q, k, v, lam):
    s = jnp.einsum('bqhcd,bkhcd->bhcqk', q, k, preferred_element_type=jnp.float32) * (DIFF_DH ** -0.5)
    p = jax.nn.softmax(s, axis=-1)
    a = p[:, :, 0] - lam * p[:, :, 1]
    return jnp.einsum('bhqk,bkhe->bqhe', a.astype(v.dtype), v, preferred_element_type=jnp.float32)


def diff_head_norm(o, g, lam_init, dtype):
    ms = jnp.mean(jnp.square(o), axis=-1, keepdims=True)
    return (o * lax.rsqrt(ms + RMS_EPS) * g * (1.0 - lam_init)).astype(dtype)


def diff_attention_latent(q, k_all, v_all, lam):
    b, l, h, _, dh = q.shape
    nb = l // Q_BLOCK
    qb = q.reshape(b, nb, Q_BLOCK, h, 2, dh).swapaxes(0, 1)
    ob = lax.map(lambda qi: diff_attend(qi, k_all, v_all, lam), qb)
    return ob.swapaxes(0, 1).reshape(b, l, h, DIFF_VDIM)


def even_mixer(h_lat, h_ctx, w_in, w_conv, lam, sub_g, lam_init, w_o, need_ctx):
    b, l, _ = h_lat.shape
    lc = h_ctx.shape[1]
    dt = h_lat.dtype
    a_end = 3 * A_WIDTH
    if need_ctx:
        pa_ctx, q_ctx, kv_ctx = jnp.split(h_ctx @ w_in, [a_end, a_end + DIFF_QK], axis=-1)
    else:
        kv_ctx = h_ctx @ w_in[:, a_end + DIFF_QK:]
    k_ctx, v_ctx = jnp.split(kv_ctx, [DIFF_QK], axis=-1)
    k_ctx = k_ctx.reshape(b, lc, DIFF_HEADS, 2, DIFF_DH)
    v_ctx = v_ctx.reshape(b, lc, DIFF_HEADS, DIFF_VDIM)
    pa, q, k, v = jnp.split(h_lat @ w_in, [a_end, a_end + DIFF_QK, a_end + 2 * DIFF_QK], axis=-1)
    cos, sin = axial_rope(l, DIFF_DH)
    q = apply_rope(q.reshape(b, l, DIFF_HEADS, 2, DIFF_DH), cos, sin)
    k = apply_rope(k.reshape(b, l, DIFF_HEADS, 2, DIFF_DH), cos, sin)
    v = v.reshape(b, l, DIFF_HEADS, DIFF_VDIM)
    k_all = jnp.concatenate([k_ctx, k], axis=1)
    v_all = jnp.concatenate([v_ctx, v], axis=1)
    o = diff_attention_latent(q, k_all, v_all, lam)
    o = diff_head_norm(o, sub_g, lam_init, dt).reshape(b, l, B_WIDTH)
    y_lat = jnp.concatenate([short_conv_mixer(pa, w_conv), o], axis=-1) @ w_o
    y_ctx = None
    if need_ctx:
        o_c = diff_attend(q_ctx.reshape(b, lc, DIFF_HEADS, 2, DIFF_DH), k_ctx, v_ctx, lam)
        o_c = diff_head_norm(o_c, sub_g, lam_init, dt).reshape(b, lc, B_WIDTH)
        y_ctx = jnp.concatenate([short_conv_mixer(pa_ctx, w_conv), o_c], axis=-1) @ w_o
    return y_lat, y_ctx


def fourier_mix(u):
    b, l, _ = u.shape
    g = u.astype(jnp.float32).reshape(b, l, C_GROUPS, C_GROUP_DIM)
    mu = jnp.mean(g, axis=-1, keepdims=True)
    var = jnp.mean(jnp.square(g - mu), axis=-1, keepdims=True)
    g = (g - mu) * lax.rsqrt(var + LN_EPS)
    f = jnp.real(jnp.fft.fft2(g, axes=(1, 3), norm='ortho'))
    return f.reshape(b, l, C_WIDTH).astype(u.dtype)


def neighbourhood_attention(q, k, v, k_ctx, v_ctx, rpb):
    b, l, _ = q.shape
    rows = l // GRID_W
    kr = min(NA_KR_MAX, rows)
    h, dh = NA_HEADS, NA_DH
    scale = dh ** -0.5
    qg = q.reshape(b, rows, GRID_W, h, dh)
    kg = k.reshape(b, rows, GRID_W, h, dh)
    vg = v.reshape(b, rows, GRID_W, h, dh)
    cols = jnp.arange(GRID_W)
    col_start = jnp.clip(cols - NA_KC // 2, 0, GRID_W - NA_KC)
    col_valid = (cols[None, :] >= col_start[:, None]) & (cols[None, :] < col_start[:, None] + NA_KC)
    dc_idx = jnp.clip(cols[None, :] - cols[:, None] + NA_KC - 1, 0, 2 * NA_KC - 2)
    rpb_c = rpb[:, :, dc_idx].astype(jnp.float32)

    def row_fn(r):
        rs = jnp.clip(r - kr // 2, 0, rows - kr)
        q_r = lax.dynamic_index_in_dim(qg, r, axis=1, keepdims=False)
        k_r = lax.dynamic_slice_in_dim(kg, rs, kr, axis=1)
        v_r = lax.dynamic_slice_in_dim(vg, rs, kr, axis=1)
        dr_idx = rs + jnp.arange(kr) - r + NA_KR_MAX - 1
        bias = jnp.take(rpb_c, dr_idx, axis=1).transpose(0, 2, 1, 3)
        s_loc = jnp.einsum('bqhd,bjkhd->bhqjk', q_r, k_r, preferred_element_type=jnp.float32) * scale
        s_loc = jnp.where(col_valid[:, None, :], s_loc + bias[None], NEG_INF)
        s_ctx = jnp.einsum('bqhd,bkhd->bhqk', q_r, k_ctx, preferred_element_type=jnp.float32) * scale
        s = jnp.concatenate([s_loc.reshape(b, h, GRID_W, kr * GRID_W), s_ctx], axis=-1)
        p = jax.nn.softmax(s, axis=-1).astype(v.dtype)
        p_loc = p[..., :kr * GRID_W].reshape(b, h, GRID_W, kr, GRID_W)
        p_ctx = p[..., kr * GRID_W:]
        return (jnp.einsum('bhqjk,bjkhd->bqhd', p_loc, v_r, preferred_element_type=jnp.float32)
                + jnp.einsum('bhqk,bkhd->bqhd', p_ctx, v_ctx, preferred_element_type=jnp.float32))

    o = lax.map(row_fn, jnp.arange(rows))
    return o.transpose(1, 0, 2, 3, 4).reshape(b, l, h * dh).astype(q.dtype)


def odd_mixer(h_lat, h_ctx, w_in, rpb, w_o, need_ctx):
    b, l, _ = h_lat.shape
    lc = h_ctx.shape[1]
    if need_ctx:
        pc_ctx, q_ctx, kv_ctx = jnp.split(h_ctx @ w_in, [C_WIDTH, C_WIDTH + D_WIDTH], axis=-1)
    else:
        kv_ctx = h_ctx @ w_in[:, C_WIDTH + D_WIDTH:]
    k_ctx, v_ctx = jnp.split(kv_ctx, [D_WIDTH], axis=-1)
    k_ctx = k_ctx.reshape(b, lc, NA_HEADS, NA_DH)
    v_ctx = v_ctx.reshape(b, lc, NA_HEADS, NA_DH)
    pc, q, k, v = jnp.split(h_lat @ w_in, [C_WIDTH, C_WIDTH + D_WIDTH, C_WIDTH + 2 * D_WIDTH], axis=-1)
    o = neighbourhood_attention(q, k, v, k_ctx, v_ctx, rpb)
    y_lat = jnp.concatenate([fourier_mix(pc), o], axis=-1) @ w_o
    y_ctx = None
    if need_ctx:
        qc = q_ctx.reshape(b, lc, NA_HEADS, NA_DH)
        s = jnp.einsum('bqhd,bkhd->bhqk', qc, k_ctx, preferred_element_type=jnp.float32) * (NA_DH ** -0.5)
        p = jax.nn.softmax(s, axis=-1).astype(v_ctx.dtype)
        o_c = jnp.einsum('bhqk,bkhd->bqhd', p, v_ctx).reshape(b, lc, D_WIDTH)
        y_ctx = jnp.concatenate([fourier_mix(pc_ctx), o_c], axis=-1) @ w_o
    return y_lat, y_ctx


def swiglu(h, w_gate, w_up, w_down):
    return (jax.nn.silu(h @ w_gate) * (h @ w_up)) @ w_down


def moe_swiglu(h, w_router, w_gate, w_up, w_down):
    logits = jnp.einsum('bld,de->ble', h, w_router, preferred_element_type=jnp.float32)
    top_v, top_i = lax.top_k(logits, TOP_K)
    top_p = jax.nn.softmax(top_v, axis=-1)
    gates = jnp.sum(jax.nn.one_hot(top_i, N_EXPERTS, dtype=jnp.float32) * top_p[..., None], axis=-2).astype(h.dtype)
    out = jnp.zeros_like(h)
    for e in range(N_EXPERTS):
        out = out + gates[..., e:e + 1] * swiglu(h, w_gate[e], w_up[e], w_down[e])
    return out


def setup_inputs(seed: int = 0) -> dict:
    key = jax.random.key(seed)
    ks = jax.random.split(key, 32)
    d = D_MODEL
    beta = DEEPNORM_BETA

    def nrm(k, shape, scale):
        return jax.random.normal(k, shape, jnp.float32) * scale

    return {
        'x': nrm(ks[0], (BATCH, SEQ, d), 1.0),
        'c': nrm(ks[1], (BATCH, d), 1.0),
        'ctx': nrm(ks[2], (BATCH, CTX_LEN, d), 1.0),
        'c_ctx': nrm(ks[3], (d,), 1.0),
        'w_mod': nrm(ks[4], (DEPTH, d, 6 * d), d ** -0.5),
        'b_mod': nrm(ks[5], (DEPTH, 6 * d), 0.02),
        'ln_g': 1.0 + nrm(ks[6], (DEPTH, 2, d), 0.02),
        'ln_b': nrm(ks[7], (DEPTH, 2, d), 0.02),
        'e_w_in': nrm(ks[8], (N_EVEN, d, EVEN_IN), d ** -0.5),
        'e_conv': nrm(ks[9], (N_EVEN, 3, A_WIDTH), 3 ** -0.5),
        'e_lam_q1': nrm(ks[10], (N_EVEN, DIFF_DH), 0.1),
        'e_lam_k1': nrm(ks[11], (N_EVEN, DIFF_DH), 0.1),
        'e_lam_q2': nrm(ks[12], (N_EVEN, DIFF_DH), 0.1),
        'e_lam_k2': nrm(ks[13], (N_EVEN, DIFF_DH), 0.1),
        'e_subln_g': 1.0 + nrm(ks[14], (N_EVEN, DIFF_VDIM), 0.02),
        'e_w_o': nrm(ks[15], (N_EVEN, EVEN_MIX, d), beta * EVEN_MIX ** -0.5),
        'e_ffn_gate': nrm(ks[16], (N_EVEN, d, D_FF), d ** -0.5),
        'e_ffn_up': nrm(ks[17], (N_EVEN, d, D_FF), d ** -0.5),
        'e_ffn_down': nrm(ks[18], (N_EVEN, D_FF, d), beta * D_FF ** -0.5),
        'o_w_in': nrm(ks[19], (N_ODD, d, ODD_IN), d ** -0.5),
        'o_rpb': nrm(ks[20], (N_ODD, NA_HEADS, 2 * NA_KR_MAX - 1, 2 * NA_KC - 1), 0.1),
        'o_w_o': nrm(ks[21], (N_ODD, ODD_MIX, d), beta * ODD_MIX ** -0.5),
        'o_router': nrm(ks[22], (N_ODD, d, N_EXPERTS), d ** -0.5),
        'o_exp_gate': nrm(ks[23], (N_ODD, N_EXPERTS, d, D_FF_EXPERT), d ** -0.5),
        'o_exp_up': nrm(ks[24], (N_ODD, N_EXPERTS, d, D_FF_EXPERT), d ** -0.5),
        'o_exp_down': nrm(ks[25], (N_ODD, N_EXPERTS, D_FF_EXPERT, d), beta * D_FF_EXPERT ** -0.5),
    }


def reference(x, c, ctx, c_ctx, w_mod, b_mod, ln_g, ln_b,
              e_w_in, e_conv, e_lam_q1, e_lam_k1, e_lam_q2, e_lam_k2, e_subln_g, e_w_o,
              e_ffn_gate, e_ffn_up, e_ffn_down,
              o_w_in, o_rpb, o_w_o, o_router, o_exp_gate, o_exp_up, o_exp_down):
    alpha = DEEPNORM_ALPHA
    x_lat, x_ctx = x, ctx
    for i in range(DEPTH):
        j = i // 2
        even = i % 2 == 0
        need_ctx = i < DEPTH - 1
        sh1, sc1, g1, sh2, sc2, g2 = adaln(c, w_mod[i], b_mod[i])
        csh1, csc1, cg1, csh2, csc2, cg2 = adaln(c_ctx, w_mod[i], b_mod[i])

        h_lat = modulate(x_lat, sh1, sc1)
        h_ctx = modulate(x_ctx, csh1, csc1)
        if even:
            lam_init = 0.8 - 0.6 * math.exp(-0.3 * i)
            f32 = jnp.float32
            lam = (jnp.exp(jnp.sum(e_lam_q1[j].astype(f32) * e_lam_k1[j].astype(f32)))
                   - jnp.exp(jnp.sum(e_lam_q2[j].astype(f32) * e_lam_k2[j].astype(f32))) + lam_init)
            y_lat, y_ctx = even_mixer(h_lat, h_ctx, e_w_in[j], e_conv[j], lam, e_subln_g[j], lam_init,
                                      e_w_o[j], need_ctx)
        else:
            y_lat, y_ctx = odd_mixer(h_lat, h_ctx, o_w_in[j], o_rpb[j], o_w_o[j], need_ctx)
        x_lat = layer_norm(alpha * x_lat + g1 * y_lat, ln_g[i, 0], ln_b[i, 0])

        h_lat = modulate(x_lat, sh2, sc2)
        if even:
            f_lat = swiglu(h_lat, e_ffn_gate[j], e_ffn_up[j], e_ffn_down[j])
        else:
            f_lat = moe_swiglu(h_lat, o_router[j], o_exp_gate[j], o_exp_up[j], o_exp_down[j])
        x_lat = layer_norm(alpha * x_lat + g2 * f_lat, ln_g[i, 1], ln_b[i, 1])

        if need_ctx:
            x_ctx = layer_norm(alpha * x_ctx + cg1 * y_ctx, ln_g[i, 0], ln_b[i, 0])
            h_ctx = modulate(x_ctx, csh2, csc2)
            if even:
                f_ctx = swiglu(h_ctx, e_ffn_gate[j], e_ffn_up[j], e_ffn_down[j])
            else:
                f_ctx = moe_swiglu(h_ctx, o_router[j], o_exp_gate[j], o_exp_up[j], o_exp_down[j])
            x_ctx = layer_norm(alpha * x_ctx + cg2 * f_ctx, ln_g[i, 1], ln_b[i, 1])
    return x_lat
```

```python
import contextlib
import math
import numpy as np
import ml_dtypes
import concourse.bass as bass
import concourse.mybir as mybir
from concourse.ap import AP
from concourse.bass_utils import run_bass_kernel_spmd

F32 = mybir.dt.float32
BF16 = mybir.dt.bfloat16
AF = mybir.ActivationFunctionType
ALU = mybir.AluOpType
AX = mybir.AxisListType

D = 1024
L = 2048
LC = 256
TT = L + LC
NT = TT // 128
DEPTH = 2
ALPHA = (2 * DEPTH) ** 0.25
LN_EPS = 1e-5
RMS_EPS = 1e-5
LAM_INIT0 = 0.8 - 0.6 * math.exp(-0.3 * 0)
D_FF = 2816
N_EXP = 8
D_FFE = 3584
GRID_W = 64


class Sched:
    ENGS = ("pe", "act", "dve", "pool", "sp")

    def __init__(self, nc):
        self.nc = nc
        self.ops = []

    def op(self, eng, fn, reads=(), writes=()):
        self.ops.append(dict(eng=eng, fn=fn, reads=tuple(reads), writes=tuple(writes),
                             dma=False, semkey=None, barrier=False))

    def dma(self, queue, fn, reads=(), writes=(), semkey=None):
        assert semkey is not None
        self.ops.append(dict(eng=queue, fn=fn, reads=tuple(reads), writes=tuple(writes),
                             dma=True, semkey=semkey, barrier=False))

    def capture(self):
        self._saved = self.ops
        self.ops = []

    def end_capture(self):
        c = self.ops
        self.ops = self._saved
        return c

    def extend_interleaved(self, lists):
        n = max(len(l) for l in lists)
        for i in range(n):
            for l in lists:
                if i < len(l):
                    self.ops.append(l[i])

    def barrier(self, fn):
        self.ops.append(dict(eng="dve", fn=fn, reads=(), writes=(), dma=False, semkey=None,
                             barrier=True))

    def run(self):
        nc = self.nc
        ops = self.ops
        n = len(ops)
        last_w = {}
        readers = {}
        deps = [None] * n
        last_on_eng = {}
        dma_since_barrier = []
        last_barrier = None
        for i, o in enumerate(ops):
            if o["barrier"]:
                keep = set(last_on_eng.values()) | set(dma_since_barrier)
                if last_barrier is not None:
                    keep.add(last_barrier)
                deps[i] = keep
                last_barrier = i
                dma_since_barrier = []
                last_on_eng = {}
                last_w = {}
                readers = {}
                continue
            d = set()
            rd = list(o["reads"])
            wr = list(o["writes"])
            ps_r = [r for r in rd if isinstance(r, tuple) and r and r[0] == "ps"]
            rd = [r for r in rd if r not in ps_r]
            wr = wr + ps_r
            for r in rd:
                if r in last_w:
                    d.add((last_w[r], "raw"))
            for w in wr:
                if w in last_w:
                    d.add((last_w[w], "waw"))
                for q in readers.get(w, ()):
                    d.add((q, "war"))
            for r in rd:
                readers.setdefault(r, []).append(i)
            for w in wr:
                last_w[w] = i
                readers[w] = []
            keep = set()
            for (j, kind) in d:
                if j == i:
                    continue
                pj = ops[j]
                if pj["dma"] or o["dma"]:
                    keep.add(j)
                    continue
                if pj["eng"] == o["eng"]:
                    if o["eng"] == "pe":
                        continue
                    keep.add(j)
                    continue
                keep.add(j)
            if last_barrier is not None:
                keep.add(last_barrier)
            deps[i] = keep
            if o["dma"]:
                dma_since_barrier.append(i)
            else:
                last_on_eng[o["eng"]] = i
        needed = [False] * n
        for i in range(n):
            for j in deps[i]:
                needed[j] = True
        sig = [None] * n
        cnt = {}
        for i, o in enumerate(ops):
            if o["dma"]:
                key = ("dma", o["semkey"])
                cnt[key] = cnt.get(key, 0) + 16
                sig[i] = (key, cnt[key], 16)
            elif needed[i]:
                key = ("eng", o["eng"])
                cnt[key] = cnt.get(key, 0) + 1
                sig[i] = (key, cnt[key], 1)
        streams = {e: [] for e in self.ENGS}
        waited = {e: {} for e in self.ENGS}
        issued = {}
        for i, o in enumerate(ops):
            e = o["eng"]
            w = {}
            for j in deps[i]:
                key, val, _ = sig[j]
                if val > w.get(key, 0):
                    w[key] = val
            for key, val in w.items():
                if key[0] == "dma" and issued.get(key, 0) > val and not (o["dma"] and ("dma", o["semkey"]) == key):
                    print("SCHED WARNING: partial DMA-sem wait", key, val, issued[key], "op", i, o["eng"])
            if o["dma"]:
                issued[("dma", o["semkey"])] = sig[i][1]
            wl = []
            for key, val in w.items():
                if waited[e].get(key, 0) >= val:
                    continue
                waited[e][key] = val
                wl.append((key, val))
            streams[e].append((i, wl))
        with contextlib.ExitStack() as st:
            sems = {}
            for s in sig:
                if s is not None and s[0] not in sems:
                    sems[s[0]] = st.enter_context(nc.semaphore("s%d" % len(sems)))
            self.n_sems = len(sems)
            block = st.enter_context(nc.Block())

            def make(engname):
                def body(eng):
                    for (i, wl) in streams[engname]:
                        for key, val in wl:
                            eng.wait_ge(sems[key], val)
                        inst = ops[i]["fn"](eng)
                        s = sig[i]
                        if s is not None:
                            assert inst is not None, "op %d returned no instruction" % i
                            inst.then_inc(sems[s[0]], s[2])
                return body

            block.tensor(make("pe"))
            block.scalar(make("act"))
            block.vector(make("dve"))
            block.gpsimd(make("pool"))
            block.sync(make("sp"))


def host_consts():
    c = {}
    c["ident"] = np.eye(128, dtype=np.float32).astype(ml_dtypes.bfloat16)
    c["ones"] = np.ones((128, 128), dtype=np.float32).astype(ml_dtypes.bfloat16)
    c["jrev"] = np.eye(128, dtype=np.float32)[::-1].copy()
    t = np.arange(L, dtype=np.int32)
    row = (t // GRID_W).astype(np.float32)
    col = (t % GRID_W).astype(np.float32)
    nf = 16
    inv = (np.float32(10000.0) ** (-np.arange(nf, dtype=np.float32) / np.float32(nf))).astype(np.float32)
    ang = np.concatenate([row[:, None] * inv, col[:, None] * inv], axis=-1)
    cos = np.cos(ang).astype(np.float32).T
    sin = np.sin(ang).astype(np.float32).T
    cosf = np.concatenate([cos, cos, cos, cos], axis=0)
    sinf = np.concatenate([-sin, sin, -sin, sin], axis=0)
    c["ropec"] = cosf.astype(ml_dtypes.bfloat16)
    c["ropes"] = sinf.astype(ml_dtypes.bfloat16)
    n = np.arange(L, dtype=np.int64)
    ph = (np.outer(n, n) % L).astype(np.float64) * (2 * np.pi / L)
    c["dft_cl"] = np.cos(ph).astype(np.float32).astype(ml_dtypes.bfloat16)
    c["dft_sl"] = (-np.sin(ph)).astype(np.float32).astype(ml_dtypes.bfloat16)
    m = np.arange(128, dtype=np.int64)
    ph2 = (np.outer(m, m) % 128).astype(np.float64) * (2 * np.pi / 128)
    c["dft_cc"] = np.cos(ph2).astype(np.float32).astype(ml_dtypes.bfloat16)
    c["dft_sc"] = np.sin(ph2).astype(np.float32).astype(ml_dtypes.bfloat16)
    qc = np.arange(64)
    cs = np.clip(qc - 8, 0, 48)
    kc = np.arange(64)
    valid = ((kc[:, None] >= cs[None, :]) & (kc[:, None] < cs[None, :] + 16)).astype(np.float32)
    c["navalid"] = np.concatenate([valid, valid], axis=0)
    return c


CONST_DT = dict(ident=BF16, ones=BF16, jrev=F32, ropec=BF16, ropes=BF16, dft_cl=BF16, dft_sl=BF16,
                dft_cc=BF16, dft_sc=BF16, navalid=F32)

W_SHAPES = dict(
    w_mod=(2, 1024, 6144), b_mod=(2, 6144), ln_g=(2, 2, 1024), ln_b=(2, 2, 1024),
    e_w_in=(1, 1024, 3072), e_conv=(1, 3, 512), e_lam_q1=(1, 64), e_lam_k1=(1, 64),
    e_lam_q2=(1, 64), e_lam_k2=(1, 64), e_subln_g=(1, 128), e_w_o=(1, 1024, 1024),
    e_ffn_gate=(1, 1024, 2816), e_ffn_up=(1, 1024, 2816), e_ffn_down=(1, 2816, 1024),
    o_w_in=(1, 1024, 2048), o_w_o=(1, 1024, 1024),
    o_exp_gate=(1, 8, 1024, 3584), o_exp_up=(1, 8, 1024, 3584), o_exp_down=(1, 8, 3584, 1024),
    c_ctx=(1024,), o_router=(1, 1024, 8),
)


def flat_ap(t, offset, dims):
    return AP(t, offset, [list(d) for d in dims])


class K:
    def __init__(self, NB=2, stop=None, dumps=()):
        self.NB = NB
        self.R = NB + 1
        self.stop = stop
        self.dumps = set(dumps)
        self.dump_specs = {}
        self.nc = nc = bass.Bass("TRN2", target_bir_lowering=False)
        self.S = Sched(nc)
        self.T = {}
        T = self.T
        T["x"] = nc.dram_tensor("x", [NB, L, D], F32, kind="ExternalInput")
        T["c"] = nc.dram_tensor("c", [NB, D], F32, kind="ExternalInput")
        T["ctx"] = nc.dram_tensor("ctx", [NB, LC, D], F32, kind="ExternalInput")
        for k, shp in W_SHAPES.items():
            T[k] = nc.dram_tensor(k, list(shp), F32, kind="ExternalInput")
        T["rpbf"] = nc.dram_tensor("rpbf", [8, 15 * 31], F32, kind="ExternalInput")
        T["routerT"] = nc.dram_tensor("routerT", [8, 1024], F32, kind="ExternalInput")
        hc = host_consts()
        for k, v in hc.items():
            T[k] = nc.dram_tensor(k, list(v.shape), CONST_DT[k], kind="ExternalInput")
        T["out"] = nc.dram_tensor("out", [NB, L, D], F32, kind="ExternalOutput")
        T["modv"] = nc.dram_tensor("modv", [2, self.R, 6144], F32, kind="Internal")
        T["xres"] = nc.dram_tensor("xres", [NB, TT, D], F32, kind="Internal")
        T["lamd"] = nc.dram_tensor("lamd", [1, 8], F32, kind="Internal")
        T["rpbp"] = nc.dram_tensor("rpbp", [8, 640], F32, kind="Internal")
        self._bank_rr = {}
        self._uid = 0

    def uid(self):
        self._uid += 1
        return self._uid

    def view(self, off, shape, dt):
        esz = 4 if dt == F32 else 2
        nel = int(np.prod(shape))
        nb = nel * esz
        assert off % 4 == 0 and off + nb <= self.ARENA_BYTES, (off, nb, self.ARENA_BYTES)
        a = self.arena[:, off // 2: (off + nb) // 2]
        if dt == F32:
            a = a.bitcast(F32)
        if len(shape) == 2:
            a = a.rearrange("p (a b) -> p a b", a=shape[0])
        elif len(shape) == 3:
            a = a.rearrange("p (a b c) -> p a b c", a=shape[0], b=shape[1])
        return a

    def dump(self, name, ap, shape, dt, reads):
        if name not in self.dumps:
            return
        t = self.nc.dram_tensor("dbg_" + name, list(shape), dt, kind="ExternalOutput")
        self.dump_specs[name] = (shape, dt)
        self.S.dma("sp", lambda e, t=t, ap=ap: e.dma_start(out=t.ap(), in_=ap), reads=reads,
                   writes=[("dbg", name)], semkey=("dbg", name))
        self.final_reads.append(("dbg", name))

    def phase_barrier(self):
        bt = self.bar_tile
        self.S.barrier(lambda e: e.memset(bt[:, 0:1], 0.0))

    def build(self):
        nc = self.nc
        S = self.S
        self.final_reads = []
        with contextlib.ExitStack() as st:
            self.hfm = st.enter_context(nc.sbuf_tensor("hfm", [128, 8, TT], BF16))
            self.ARENA_BYTES = 156 * 1024
            self.arena = st.enter_context(nc.sbuf_tensor("arena", [128, self.ARENA_BYTES // 2], BF16))
            self.psum = st.enter_context(nc.psum_tensor("psum", [128, 8, 512], F32))
            self.ident = st.enter_context(nc.sbuf_tensor("ident_sb", [128, 128], BF16))
            self.ones = st.enter_context(nc.sbuf_tensor("ones_sb", [128, 128], BF16))
            self.bar_tile = st.enter_context(nc.sbuf_tensor("bar", [128, 4], F32))
            self.small = st.enter_context(nc.sbuf_tensor("small", [128, 64], F32))
            self.gates = st.enter_context(nc.sbuf_tensor("gates", [128, 16, 8], F32))
            self.gw8 = st.enter_context(nc.sbuf_tensor("gw8", [128, 3, 8], F32))
            self.gsm = st.enter_context(nc.sbuf_tensor("gsm", [128, 8], F32))
            self.rts = st.enter_context(nc.sbuf_tensor("rts", [128, 2, 48], F32))
            self.nmr = st.enter_context(nc.sbuf_tensor("nmr", [128, 2], F32))
            T = self.T
            S.dma("sp", lambda e: e.dma_start(out=self.ident[:], in_=T["ident"].ap()), writes=["ident"], semkey="c_ident")
            S.dma("sp", lambda e: e.dma_start(out=self.ones[:], in_=T["ones"].ap()), writes=["ones"], semkey="c_ones")
            self.phase_mod()
            for b in range(self.NB):
                if self.stop == "mod":
                    break
                self.layer0(b)
                if self.stop and self.stop.startswith("l0"):
                    continue
                self.layer1(b)
            self.phase_barrier()
            S.op("sp", lambda e: e.nop(), reads=self.final_reads)
            S.run()
        return nc

    def phase_mod(self):
        S, T, R, NB = self.S, self.T, self.R, self.NB
        self.phase_barrier()
        cT = self.view(0, (8, R), F32)
        scT = self.view(256, (8, R), F32)
        NWM = 4
        wm = [self.view(1024 + i * 16384, (8, 512), F32) for i in range(NWM)]
        modsb = self.view(1024 + NWM * 16384, (6144,), F32)
        bmod = self.view(1024 + NWM * 16384 + 24576, (6144,), F32)
        for r in range(R):
            if r < NB:
                src = flat_ap(T["c"], r * D, [[1, 128], [128, 8], [1, 1]])
            else:
                src = flat_ap(T["c_ctx"], 0, [[1, 128], [128, 8], [1, 1]])
            S.dma("sp", lambda e, src=src, r=r: e.dma_start(out=cT[:, :, r:r + 1], in_=src,
                                                          allow_slow_non_contiguous=True),
                  writes=[("cT", r)], semkey="cT")
        S.op("act", lambda e: e.activation(out=scT[:, :, :], in_=cT[:, :, :], func=AF.Silu),
             reads=[("cT", r) for r in range(R)], writes=["scT"])
        ps = self.psum
        for i in range(2):
            S.dma("sp", lambda e, i=i: e.dma_start(out=bmod[0:R, :], in_=flat_ap(T["b_mod"], i * 6144, [[0, R], [1, 6144]])),
                  writes=["bmod"], semkey="bmod")
            for ng in range(12):
                w = wm[ng % NWM]
                wsrc = T["w_mod"].ap()[i].rearrange("(k p) n -> p k n", p=128)[:, :, ng * 512:(ng + 1) * 512]
                S.dma("sp", lambda e, w=w, wsrc=wsrc: e.dma_start(out=w[:, :, :], in_=wsrc),
                      writes=[("wm", ng % NWM)], semkey=("wm", ng % NWM))
                bank = ng % 2

                def mm(e, w=w, bank=bank):
                    for k in range(8):
                        ins = e.matmul(ps[0:R, bank, :], scT[:, k, :], w[:, k, :], start=(k == 0), stop=(k == 7))
                    return ins
                S.op("pe", mm, reads=["scT", ("wm", ng % NWM)], writes=[("ps", bank)])
                S.op("dve", lambda e, bank=bank, ng=ng: e.tensor_tensor(
                    out=modsb[0:R, ng * 512:(ng + 1) * 512], in0=ps[0:R, bank, :],
                    in1=bmod[0:R, ng * 512:(ng + 1) * 512], op=ALU.add),
                    reads=[("ps", bank), "bmod"], writes=[("modsb", ng)])
            for j in (1, 4):
                S.op("dve", lambda e, j=j: e.tensor_scalar(out=modsb[0:R, j * 1024:(j + 1) * 1024],
                                                           in0=modsb[0:R, j * 1024:(j + 1) * 1024],
                                                           scalar1=1.0, scalar2=None, op0=ALU.add),
                     reads=[("modsb", 2 * j), ("modsb", 2 * j + 1)], writes=[("modsb", 2 * j), ("modsb", 2 * j + 1)])
            S.dma("sp", lambda e, i=i: e.dma_start(out=T["modv"].ap()[i], in_=modsb[0:R, :]),
                  reads=[("modsb", g) for g in range(12)], writes=[("modv", i)], semkey="modv")
        self.dump("modsb", modsb[0:R, :], (R, 6144), F32, [("modv", 1)])
        sm = self.small
        lq = self.view(1024, (4, 64), F32)
        for n_, nm in enumerate(("e_lam_q1", "e_lam_k1", "e_lam_q2", "e_lam_k2")):
            S.dma("sp", lambda e, n_=n_, nm=nm: e.dma_start(out=lq[0:1, n_, :], in_=T[nm].ap()),
                  writes=[("lq", n_)], semkey=("lq", n_))
        junk = self.view(2048, (64,), F32)
        for h_ in range(2):
            S.op("dve", lambda e, h_=h_: e.tensor_tensor(out=junk[0:1, :], in0=lq[0:1, 2 * h_, :], in1=lq[0:1, 2 * h_ + 1, :], op=ALU.mult),
                 reads=[("lq", 2 * h_), ("lq", 2 * h_ + 1)], writes=["junk"])
            S.op("dve", lambda e, h_=h_: e.reduce_sum(out=sm[0:1, h_:h_ + 1], in_=junk[0:1, :], axis=AX.X),
                 reads=["junk"], writes=[("sm", h_)])
        S.op("act", lambda e: e.activation(out=sm[0:1, 2:4], in_=sm[0:1, 0:2], func=AF.Exp),
             reads=[("sm", 0), ("sm", 1)], writes=[("sm", 2)])
        S.op("dve", lambda e: e.tensor_tensor(out=sm[0:1, 4:5], in0=sm[0:1, 3:4], in1=sm[0:1, 2:3], op=ALU.subtract),
             reads=[("sm", 2)], writes=[("sm", 4)])
        S.op("dve", lambda e: e.tensor_scalar(out=sm[0:1, 5:6], in0=sm[0:1, 4:5], scalar1=-LAM_INIT0, scalar2=None, op0=ALU.add),
             reads=[("sm", 4)], writes=[("sm", 5)])
        S.dma("sp", lambda e: e.dma_start(out=T["lamd"].ap()[0:1, 0:1], in_=sm[0:1, 5:6]),
              reads=[("sm", 5)], writes=["lamd"], semkey="lamd")
        S.dma("sp", lambda e: e.dma_start(out=sm[:, 8:9], in_=flat_ap(T["lamd"], 0, [[0, 128], [1, 1]])),
              reads=["lamd"], writes=["neglam"], semkey="neglam")
        S.dma("sp", lambda e: e.dma_start(out=sm[:, 10:11], in_=flat_ap(T["e_subln_g"], 0, [[1, 128], [1, 1]])),
              writes=["gsc0"], semkey="gsc0")
        S.op("dve", lambda e: e.tensor_scalar(out=sm[:, 9:10], in0=sm[:, 10:11], scalar1=1.0 - LAM_INIT0, scalar2=None, op0=ALU.mult),
             reads=["gsc0"], writes=["gsc"])
        self.dump("small", sm[:, :], (128, 64), F32, ["gsc", "neglam"])

    def bc_load(self, slot_ap, key, layer, row, j):
        T = self.T
        src = flat_ap(T["modv"], (layer * self.R + row) * 6144 + j * 1024, [[0, 128], [1, 1024]])
        self.S.dma("sp", lambda e: e.dma_start(out=slot_ap, in_=src), reads=[("modv", layer)],
                   writes=[key], semkey=key)

    def bc_load_ln(self, slot_ap, key, name, layer, s):
        T = self.T
        src = flat_ap(T[name], (layer * 2 + s) * 1024, [[0, 128], [1, 1024]])
        self.S.dma("sp", lambda e: e.dma_start(out=slot_ap, in_=src), writes=[key], semkey=key)

    def modulate_transpose(self, xt, xkey, sc, sckey, sh, shkey, tmp, tmpkey, hb, hbkey, t, banks, h32=None, h32key=None):
        S = self.S
        ps = self.psum
        S.op("dve", lambda e: e.tensor_tensor(out=tmp, in0=xt, in1=sc, op=ALU.mult),
             reads=[xkey, sckey], writes=[tmpkey])
        if h32 is None:
            S.op("dve", lambda e: e.tensor_tensor(out=hb, in0=tmp, in1=sh, op=ALU.add),
                 reads=[tmpkey, shkey], writes=[hbkey])
        else:
            S.op("dve", lambda e: e.tensor_tensor(out=h32, in0=tmp, in1=sh, op=ALU.add),
                 reads=[tmpkey, shkey], writes=[h32key])
            S.op("act", lambda e: e.copy(out=hb, in_=h32), reads=[h32key], writes=[hbkey])
        for half in range(2):
            bank = banks[half]

            def tr(e, half=half, bank=bank):
                for kk in range(4):
                    k = half * 4 + kk
                    ins = e.matmul(ps[:, bank, kk * 128:(kk + 1) * 128], hb[:, k * 128:(k + 1) * 128],
                                   self.ident[:, :], start=True, stop=True)
                return ins
            S.op("pe", tr, reads=[hbkey, "ident"], writes=[("ps", bank)])
            S.op("act", lambda e, half=half, bank=bank: e.copy(
                out=self.hfm[:, half * 4:(half + 1) * 4, t * 128:(t + 1) * 128],
                in_=ps[:, bank, :].rearrange("p (a b) -> p a b", a=4)),
                reads=[("ps", bank)], writes=[("hfm", t, half)])

    def hfm_keys(self, t0, size):
        return [("hfm", t, h) for t in range(t0 // 128, (t0 + size + 127) // 128) for h in range(2)]

    def layer_norm_tile(self, tt, ttkey, lng, lngkey, lnb, lnbkey, xo, xokey, stats, mv, u):
        S = self.S
        sk = ("lnstats", u)
        for hf in range(2):
            S.op("dve", lambda e, hf=hf: e.bn_stats(out=stats[:, hf, :], in_=tt[:, hf * 512:(hf + 1) * 512]),
                 reads=[ttkey], writes=[(sk, hf)])
        S.op("dve", lambda e: e.bn_aggr(out=mv[:, 0:2], in_=stats[:, :, :]), reads=[(sk, 0), (sk, 1)], writes=[(sk, "mv")])
        ec = self.epsc(LN_EPS)
        S.op("act", lambda e: e.activation(out=mv[:, 2:3], in_=mv[:, 1:2], func=AF.Ln, bias=ec, scale=1.0),
             reads=[(sk, "mv"), self.eps_key(LN_EPS)], writes=[(sk, "ln")])
        S.op("act", lambda e: e.activation(out=mv[:, 3:4], in_=mv[:, 2:3], func=AF.Exp, scale=-0.5),
             reads=[(sk, "ln")], writes=[(sk, "rstd")])
        S.op("dve", lambda e: e.tensor_scalar(out=xo, in0=tt, scalar1=mv[:, 0:1], scalar2=mv[:, 3:4],
                                              op0=ALU.subtract, op1=ALU.mult),
             reads=[ttkey, (sk, "mv"), (sk, "rstd")], writes=[xokey])
        S.op("dve", lambda e: e.tensor_tensor(out=xo, in0=xo, in1=lng, op=ALU.mult), reads=[xokey, lngkey], writes=[xokey])
        S.op("dve", lambda e: e.tensor_tensor(out=xo, in0=xo, in1=lnb, op=ALU.add), reads=[xokey, lnbkey], writes=[xokey])

    def epsc(self, val):
        if not hasattr(self, "_eps"):
            self._eps = {}
        if val not in self._eps:
            col = 16 + len(self._eps)
            ap = self.small[:, col:col + 1]
            self.S.op("dve", lambda e: e.memset(ap, float(val)), writes=[("epsc", col)])
            self._eps[val] = (ap, ("epsc", col))
        return self._eps[val][0]

    def eps_key(self, val):
        self.epsc(val)
        return self._eps[val][1]


A_MIX = 0
A_SCR = 36864
A_ACC = 0
A_BC = 73728
A_WORK = 98304
A_HB = 122880
A_WB = 126976


def _k_common_views(self):
    self.mix = self.view(A_MIX, (8, TT), BF16)
    self.acc = self.view(A_ACC, (NT, 1024), F32)
    self.bc = [self.view(A_BC + i * 4096, (1024,), F32) for i in range(6)]
    self.work = [self.view(A_WORK + i * 4096, (1024,), F32) for i in range(6)]
    self.hb = [self.view(A_HB + i * 2048, (1024,), BF16) for i in range(2)]


K.common_views = _k_common_views


def _tile_src(self, b, t):
    T = self.T
    if t < 2:
        return T["ctx"].ap()[b, t * 128:(t + 1) * 128, :]
    return T["x"].ap()[b, (t - 2) * 128:(t - 1) * 128, :]


K.tile_src = _tile_src


def _stats_views(self, i):
    base = 24 + i * 16
    st = self.small[:, base:base + 12].rearrange("p (a b) -> p a b", a=2)
    mv = self.small[:, base + 12:base + 16]
    return st, mv


K.stats_views = _stats_views


def _tm_tile(self, b, layer, t, mode, wr_b=None, wo=None):
    S, T = self.S, self.T
    ps = self.psum
    r = t % 2
    X, Y, W = self.work[r], self.work[2 + r], self.work[4 + r]
    kX, kY, kW = ("wX", r), ("wY", r), ("wW", r)
    hb, khb = self.hb[r], ("hb", r)
    bc = self.bc
    if mode == "s1" or (mode == "s3" and layer == 0):
        src, rk = self.tile_src(b, t), []
    else:
        src, rk = T["xres"].ap()[b, t * 128:(t + 1) * 128, :], [("xres", b, t)]
    S.dma("sp", lambda e: e.dma_start(out=X, in_=src), reads=rk, writes=[kX], semkey=kX)
    if mode == "s1":
        S.op("dve", lambda e: e.tensor_tensor(out=W, in0=X, in1=bc[0], op=ALU.mult), reads=[kX, ("bc", 0)], writes=[kW])
        S.op("dve", lambda e: e.tensor_tensor(out=hb, in0=W, in1=bc[1], op=ALU.add), reads=[kW, ("bc", 1)], writes=[khb])
    else:
        if mode == "s3":
            for n_ in range(2):
                bank = 4 + 2 * r + n_

                def mmo(e, n_=n_, bank=bank):
                    for k in range(8):
                        ins = e.matmul(ps[:, bank, :], self.mix[:, k, t * 128:(t + 1) * 128], wo[:, k, n_ * 512:(n_ + 1) * 512],
                                       start=(k == 0), stop=(k == 7))
                    return ins
                S.op("pe", mmo, reads=["wo"], writes=[("ps", bank)])
                S.op("dve", lambda e, n_=n_, bank=bank: e.tensor_tensor(
                    out=Y[:, n_ * 512:(n_ + 1) * 512], in0=ps[:, bank, :], in1=bc[0][:, n_ * 512:(n_ + 1) * 512], op=ALU.mult),
                    reads=[("ps", bank), ("bc", 0)], writes=[kY])
            ykeys = [kY]
        else:
            ti = t if layer == 0 else t - 2
            S.op("dve", lambda e: e.tensor_tensor(out=Y, in0=self.acc[:, ti, :], in1=bc[0], op=ALU.mult),
                 reads=[("bc", 0)], writes=[kY])
            ykeys = [kY]
        S.op("dve", lambda e: e.scalar_tensor_tensor(out=Y, in0=X, scalar=ALPHA, in1=Y, op0=ALU.mult, op1=ALU.add),
             reads=[kX] + ykeys, writes=[kY])
        st_, mv = self.stats_views(r)
        sk = ("lnstats", r)
        for hf in range(2):
            S.op("dve", lambda e, hf=hf: e.bn_stats(out=st_[:, hf, :], in_=Y[:, hf * 512:(hf + 1) * 512]), reads=[kY], writes=[(sk, hf)])
        S.op("dve", lambda e: e.bn_aggr(out=mv[:, 0:2], in_=st_[:, :, :]), reads=[(sk, 0), (sk, 1)], writes=[(sk, "mv")])
        ec = self.epsc(LN_EPS)
        S.op("act", lambda e: e.activation(out=mv[:, 2:3], in_=mv[:, 1:2], func=AF.Ln, bias=ec, scale=1.0),
             reads=[(sk, "mv"), self.eps_key(LN_EPS)], writes=[(sk, "ln")])
        S.op("act", lambda e: e.activation(out=mv[:, 3:4], in_=mv[:, 2:3], func=AF.Exp, scale=-0.5), reads=[(sk, "ln")], writes=[(sk, "rstd")])
        nmr = self.nmr[:, r:r + 1]
        S.op("dve", lambda e: e.scalar_tensor_tensor(out=nmr, in0=mv[:, 0:1], scalar=-1.0, in1=mv[:, 3:4], op0=ALU.mult, op1=ALU.mult),
             reads=[(sk, "mv"), (sk, "rstd")], writes=[(sk, "nmr")])
        S.op("act", lambda e: e.activation(out=Y, in_=Y, func=AF.Identity, bias=nmr, scale=mv[:, 3:4]),
             reads=[kY, (sk, "rstd"), (sk, "nmr")], writes=[kY])
        S.op("dve", lambda e: e.tensor_tensor(out=Y, in0=Y, in1=bc[1], op=ALU.mult), reads=[kY, ("bc", 1)], writes=[kY])
        S.op("dve", lambda e: e.tensor_tensor(out=Y, in0=Y, in1=bc[2], op=ALU.add), reads=[kY, ("bc", 2)], writes=[kY])
        if mode == "s5" and layer == 1:
            S.dma("sp", lambda e: e.dma_start(out=T["out"].ap()[b, (t - 2) * 128:(t - 1) * 128, :], in_=Y),
                  reads=[kY], writes=[("out", b, t)], semkey=("out_st", r))
            self.final_reads.append(("out", b, t))
            return
        if mode == "s3" or t >= 2:
            S.dma("sp", lambda e: e.dma_start(out=T["xres"].ap()[b, t * 128:(t + 1) * 128, :], in_=Y),
                  reads=[kY], writes=[("xres", b, t)], semkey=("xres_st", r))
        S.op("dve", lambda e: e.tensor_tensor(out=W, in0=Y, in1=bc[3], op=ALU.mult), reads=[kY, ("bc", 3)], writes=[kW])
        if mode == "s3" and layer == 1:
            S.op("dve", lambda e: e.tensor_tensor(out=W, in0=W, in1=bc[4], op=ALU.add), reads=[kW, ("bc", 4)], writes=[kW])
            S.op("act", lambda e: e.copy(out=hb, in_=W), reads=[kW], writes=[khb])
        else:
            S.op("dve", lambda e: e.tensor_tensor(out=hb, in0=W, in1=bc[4], op=ALU.add), reads=[kW, ("bc", 4)], writes=[khb])
    for half in range(2):
        bank = 2 * r + half

        def tr(e, half=half, bank=bank):
            for kk in range(4):
                k = half * 4 + kk
                ins = e.matmul(ps[:, bank, kk * 128:(kk + 1) * 128], hb[:, k * 128:(k + 1) * 128], self.ident[:, :], start=True, stop=True)
            return ins
        S.op("pe", tr, reads=[khb, "ident"], writes=[("ps", bank)])
        S.op("act", lambda e, half=half, bank=bank: e.copy(
            out=self.hfm[:, half * 4:(half + 1) * 4, t * 128:(t + 1) * 128], in_=ps[:, bank, :].rearrange("p (a b) -> p a b", a=4)),
            reads=[("ps", bank)], writes=[("hfm", t, half)])
    if mode == "s3" and layer == 1:
        self.router_tile(W, kW, wr_b, t - 2, X, kX, r)


K.tm_tile = _tm_tile


def _tm_phase(self, b, layer, mode, wr_b=None, wo=None):
    S = self.S
    kinds = ("ctx", "lat") if (layer == 0) else ("lat",)
    for kind in kinds:
        row = self.NB if kind == "ctx" else b
        tiles = list(range(0, 2)) if kind == "ctx" else list(range(2, NT))
        if mode == "s1":
            self.bc_load(self.bc[0], ("bc", 0), 0, row, 1)
            self.bc_load(self.bc[1], ("bc", 1), 0, row, 0)
        elif mode == "s3":
            self.bc_load(self.bc[0], ("bc", 0), layer, row, 2)
            self.bc_load(self.bc[3], ("bc", 3), layer, row, 4)
            self.bc_load(self.bc[4], ("bc", 4), layer, row, 3)
        else:
            self.bc_load(self.bc[0], ("bc", 0), layer, row, 5)
            if layer == 0:
                self.bc_load(self.bc[3], ("bc", 3), 1, row, 1)
                self.bc_load(self.bc[4], ("bc", 4), 1, row, 0)
        for i in range(0, len(tiles), 2):
            lists = []
            for t in tiles[i:i + 2]:
                S.capture()
                self.tm_tile(b, layer, t, mode, wr_b=wr_b, wo=wo)
                lists.append(S.end_capture())
            S.extend_interleaved(lists)


K.tm_phase = _tm_phase


def _s1(self, b):
    self.phase_barrier()
    self.common_views()
    self.tm_phase(b, 0, "s1")
    self.dump("hfm_l0_b%d" % b, self.hfm[:, :, :], (128, 8, TT), BF16, self.hfm_keys(0, TT))


K.s1 = _s1

TOK_TILES = [(0, 256)] + [(256 + i * 512, 512) for i in range(4)]


def _load_wst(self, dst, dkey, wap, col0, ncols=128, swap=False):
    S = self.S
    wv = wap.rearrange("(k p) n -> p k n", p=128)
    if not swap:
        S.dma("pool", lambda e: e.dma_start(out=dst[:, :, 0:ncols], in_=wv[:, :, col0:col0 + ncols]),
              writes=[dkey], semkey=dkey)
    else:
        assert ncols == 128
        for blk in range(4):
            sblk = blk ^ 1
            S.dma("pool", lambda e, blk=blk, sblk=sblk: e.dma_start(
                out=dst[:, :, blk * 32:(blk + 1) * 32],
                in_=wv[:, :, col0 + sblk * 32: col0 + (sblk + 1) * 32]),
                writes=[(dkey, blk)], semkey=(dkey, blk))


K.load_wst = _load_wst


def _proj_fm(self, wst, wkeys, t0, size, bank):
    ps = self.psum

    def mm(e):
        for k in range(8):
            ins = e.matmul(ps[:, bank, 0:size], wst[:, k, :], self.hfm[:, k, t0:t0 + size],
                           start=(k == 0), stop=(k == 7))
        return ins
    self.S.op("pe", mm, reads=list(wkeys) + self.hfm_keys(t0, size), writes=[("ps", bank)])


K.proj_fm = _proj_fm


def _rsqrt_act(self, out, in_, scale, epsval, rkeys, wkey, tmp, tmpkey):
    S = self.S
    ec = self.epsc(epsval)
    S.op("act", lambda e: e.activation(out=tmp, in_=in_, func=AF.Ln, bias=ec, scale=scale),
         reads=list(rkeys) + [self.eps_key(epsval)], writes=[tmpkey])
    S.op("act", lambda e: e.activation(out=out, in_=tmp, func=AF.Exp, scale=-0.5), reads=[tmpkey], writes=[wkey])


K.rsqrt_act = _rsqrt_act


def _s2_l0(self, b):
    S, T = self.S, self.T
    ps = self.psum
    self.phase_barrier()
    self.common_views()
    mix = self.mix
    w_in = T["e_w_in"].ap()[0]
    wst = [self.view(A_WB + i * 2048, (8, 128), BF16) for i in range(4)]
    ubuf = self.view(A_SCR, (2308,), F32)
    bgs = self.view(A_SCR + 9232, (TT,), F32)
    ctmp = self.view(A_SCR + 9232 + 9216, (TT,), F32)
    cgs = [self.view(A_SCR + 9232 + 2 * 9216 + i * 2048, (512,), F32) for i in range(2)]
    wconv = self.view(A_WORK, (3, 4), F32)
    for w_ in range(3):
        S.dma("sp", lambda e, w_=w_: e.dma_start(out=wconv[:, w_, :], in_=flat_ap(T["e_conv"], w_ * 512, [[1, 128], [128, 4]]),
                                                 allow_slow_non_contiguous=True), writes=[("wconv", w_)], semkey=("wconv", w_))
    for c0 in (0, 257, 258, 2307):
        S.op("dve", lambda e, c0=c0: e.memset(ubuf[:, c0:c0 + 1], 0.0), writes=[("upad", c0)])
    segs = [(0, 256, 1), (256, 2048, 259)]

    def ucol(t0):
        return 1 + t0 if t0 < 256 else 259 + (t0 - 256)
    for j in range(4):
        for which, colbase in (("bg", 0), ("cg", 512), ("val", 1024)):
            pass
        self.load_wst(wst[0], ("wst", 0), w_in, 0 + j * 128)
        self.load_wst(wst[1], ("wst", 1), w_in, 512 + j * 128)
        self.load_wst(wst[2], ("wst", 2), w_in, 1024 + j * 128)
        for ti, (t0, size) in enumerate(TOK_TILES):
            r = ti % 2
            self.proj_fm(wst[1], [("wst", 1)], t0, size, 0 + r)
            S.op("act", lambda e, r=r, size=size: e.copy(out=cgs[r][:, 0:size], in_=ps[:, 0 + r, 0:size]),
                 reads=[("ps", 0 + r)], writes=[("cgs", r)])
            self.proj_fm(wst[2], [("wst", 2)], t0, size, 2 + r)
            uc = ucol(t0)
            S.op("dve", lambda e, r=r, size=size, uc=uc: e.tensor_tensor(
                out=ubuf[:, uc:uc + size], in0=ps[:, 2 + r, 0:size], in1=cgs[r][:, 0:size], op=ALU.mult),
                reads=[("ps", 2 + r), ("cgs", r)], writes=[("ubuf", ti)])
            self.proj_fm(wst[0], [("wst", 0)], t0, size, 4 + r)
            S.op("act", lambda e, r=r, size=size, t0=t0: e.copy(out=bgs[:, t0:t0 + size], in_=ps[:, 4 + r, 0:size]),
                 reads=[("ps", 4 + r)], writes=[("bgs", ti)])
        for si, (t0, nt, uc) in enumerate(segs):
            ukeys = [("ubuf", ti) for ti in range(5)] + [("upad", c0) for c0 in (0, 257, 258, 2307)]
            ck = ("ctmp", si)
            S.op("dve", lambda e, t0=t0, nt=nt, uc=uc, j=j: e.tensor_scalar(
                out=ctmp[:, t0:t0 + nt], in0=ubuf[:, uc - 1:uc - 1 + nt], scalar1=wconv[:, 0, j:j + 1], scalar2=None,
                op0=ALU.mult), reads=ukeys + [("wconv", 0)], writes=[ck])
            for w_ in (1, 2):
                S.op("dve", lambda e, t0=t0, nt=nt, uc=uc, j=j, w_=w_: e.scalar_tensor_tensor(
                    out=ctmp[:, t0:t0 + nt], in0=ubuf[:, uc - 1 + w_:uc - 1 + w_ + nt], scalar=wconv[:, w_, j:j + 1],
                    in1=ctmp[:, t0:t0 + nt], op0=ALU.mult, op1=ALU.add), reads=ukeys + [("wconv", w_), ck], writes=[ck])
            S.op("dve", lambda e, t0=t0, nt=nt, j=j: e.tensor_tensor(
                out=mix[:, j, t0:t0 + nt], in0=ctmp[:, t0:t0 + nt], in1=bgs[:, t0:t0 + nt], op=ALU.mult),
                reads=[ck] + [("bgs", ti) for ti in range(5)], writes=[("mix", j, si)])
    self.dump("mixconv_b%d" % b, mix[:, 0:4, :], (128, 4, TT), BF16, [("mix", j, si) for j in range(4) for si in range(2)])
    if self.stop == "l0conv":
        return
    self.phase_barrier()
    vtm = self.view(A_SCR, (NT, 512), BF16)
    qr = self.view(A_SCR + 18432, (TT,), BF16)
    kz = [self.view(A_SCR + 18432 + 4608 + i * 4608, (TT,), BF16) for i in range(2)]
    Et = [self.view(A_SCR + 32256 + i * 1024, (512,), BF16) for i in range(4)]
    osq = self.view(A_WORK + 20480, (512,), BF16)
    esb = [self.view(A_WORK + 20480 + 1024 + i * 1024, (512,), BF16) for i in range(2)]
    wk32p = [self.view(A_HB + i * 2048, (512,), F32) for i in range(2)]
    S.op("dve", lambda e: e.memset(kz[0][64:128, :], 0.0), writes=[("kzz", 0)])
    S.op("dve", lambda e: e.memset(kz[1][0:64, :], 0.0), writes=[("kzz", 1)])
    ropec = self.view(A_BC, (L,), BF16)
    ropes = self.view(A_BC + 4096, (L,), BF16)
    wv = self.view(A_WB + 8192, (8, 512), BF16)
    wk32 = [self.view(A_WORK + i * 2048, (512,), F32) for i in range(10)]
    S.dma("sp", lambda e: e.dma_start(out=ropec, in_=T["ropec"].ap()), writes=["ropec"], semkey="ropec")
    S.dma("sp", lambda e: e.dma_start(out=ropes, in_=T["ropes"].ap()), writes=["ropes"], semkey="ropes")
    self.load_wst(wv, "wv", w_in, 2560, ncols=512)
    for t in range(NT):
        bank = t % 2

        def mmv(e, t=t, bank=bank):
            for k in range(8):
                ins = e.matmul(ps[:, bank, :], self.hfm[:, k, t * 128:(t + 1) * 128], wv[:, k, :],
                               start=(k == 0), stop=(k == 7))
            return ins
        S.op("pe", mmv, reads=["wv"] + self.hfm_keys(t * 128, 128), writes=[("ps", bank)])
        S.op("act", lambda e, t=t, bank=bank: e.copy(out=vtm[:, t, :], in_=ps[:, bank, :]),
             reads=[("ps", bank)], writes=[("vtm", t)])
    neglam = self.small[:, 8:9]
    gsc = self.small[:, 9:10]
    for hd in range(4):
        for (nm, col0, dst) in (("q", 1536 + hd * 128, qr), ("k", 2048 + hd * 128, None)):
            self.load_wst(wst[0], ("wst", 0), w_in, col0)
            self.load_wst(wst[1], ("wst", 1), w_in, col0, swap=True)
            swk = [(("wst", 1), blk) for blk in range(4)]
            for ti, (t0, size) in enumerate(TOK_TILES):
                r = ti % 2
                self.proj_fm(wst[0], [("wst", 0)], t0, size, 0 + r)
                if t0 < 256:
                    if nm == "q":
                        S.op("act", lambda e, r=r, size=size, t0=t0, dst=dst: e.copy(out=dst[:, t0:t0 + size], in_=ps[:, r, 0:size]),
                             reads=[("ps", r)], writes=[(nm, ti)])
                    else:
                        for c_ in range(2):
                            S.op("act", lambda e, r=r, size=size, t0=t0, c_=c_: e.copy(
                                out=kz[c_][c_ * 64:(c_ + 1) * 64, t0:t0 + size], in_=ps[c_ * 64:(c_ + 1) * 64, r, 0:size]),
                                reads=[("ps", r)], writes=[("k", ti, c_)])
                    continue
                self.proj_fm(wst[1], swk, t0, size, 2 + r)
                l0 = t0 - 256
                a1, a2 = wk32[2 + r], wk32[4 + r]
                S.op("dve", lambda e, r=r, l0=l0, a1=a1: e.tensor_tensor(out=a1, in0=ps[:, r, :], in1=ropec[:, l0:l0 + 512], op=ALU.mult),
                     reads=[("ps", r), "ropec"], writes=[("rp1", r)])
                S.op("dve", lambda e, r=r, l0=l0, a2=a2: e.tensor_tensor(out=a2, in0=ps[:, 2 + r, :], in1=ropes[:, l0:l0 + 512], op=ALU.mult),
                     reads=[("ps", 2 + r), "ropes"], writes=[("rz", r)])
                if nm == "q":
                    S.op("dve", lambda e, a1=a1, a2=a2, t0=t0, dst=dst: e.tensor_tensor(out=dst[:, t0:t0 + 512], in0=a1, in1=a2, op=ALU.add),
                         reads=[("rp1", r), ("rz", r)], writes=[(nm, ti)])
                else:
                    for c_ in range(2):
                        S.op("dve", lambda e, a1=a1, a2=a2, t0=t0, c_=c_: e.tensor_tensor(
                            out=kz[c_][c_ * 64:(c_ + 1) * 64, t0:t0 + 512], in0=a1[c_ * 64:(c_ + 1) * 64, :], in1=a2[c_ * 64:(c_ + 1) * 64, :], op=ALU.add),
                            reads=[("rp1", r), ("rz", r)], writes=[("k", ti, c_)])
        qkeys = lambda ti: [("q", ti)]
        allk = lambda c_: [("k", ti, c_) for ti in range(5)] + [("kzz", c_)]
        iters = []
        for ti, (t0, size) in enumerate(TOK_TILES):
            nchunks = 2 if t0 < 256 else NT
            for c in range(2):
                for j in range(nchunks):
                    iters.append((ti, t0, size, c, j, nchunks))
        sbanks = (0, 1, 3)

        def front(n, hd=hd):
            ti, t0, size, c, j, nchunks = iters[n]
            sb = sbanks[n % 3]
            pl, ph = c * 64, (c + 1) * 64
            S.op("pe", lambda e: e.matmul(ps[:, sb, 0:size], kz[c][:, j * 128:(j + 1) * 128], qr[:, t0:t0 + size], start=True, stop=True),
                 reads=allk(c) + qkeys(ti), writes=[("ps", sb)])

        def back(n, hd=hd):
            ti, t0, size, c, j, nchunks = iters[n]
            sb = sbanks[n % 3]
            es = n % 4
            bo, bz = 4 + 2 * c, 5 + 2 * c
            S.op("act", lambda e: e.activation(out=Et[es][:, 0:size], in_=ps[:, sb, 0:size], func=AF.Exp, scale=0.125),
                 reads=[("ps", sb)], writes=[("Et", es)])

            S.op("pe", lambda e: e.matmul(ps[:, bo, 0:size], vtm[:, j, hd * 128:(hd + 1) * 128], Et[es][:, 0:size],
                                          start=(j == 0), stop=(j == nchunks - 1)),
                 reads=[("Et", es), ("vtm", j)], writes=[("ps", bo)])
            par = j % 2
            eng = "dve"
            esum = wk32[2 * par + c] if par == 0 else wk32p[c]
            ek = ("esum", par, c)
            if j < 2:
                S.op(eng, lambda e: e.tensor_copy(out=esum[:, 0:size], in_=Et[es][:, 0:size]), reads=[("Et", es)], writes=[ek])
            else:
                S.op(eng, lambda e: e.tensor_tensor(out=esum[:, 0:size], in0=esum[:, 0:size], in1=Et[es][:, 0:size], op=ALU.add),
                     reads=[("Et", es), ek], writes=[ek])
            if j < nchunks - 1:
                return
            S.op("dve", lambda e: e.tensor_tensor(out=esb[c][:, 0:size], in0=wk32[c][:, 0:size], in1=wk32p[c][:, 0:size], op=ALU.add),
                 reads=[("esum", 0, c), ("esum", 1, c)], writes=[("esb", c)])
            S.op("pe", lambda e: e.matmul(ps[:, bz, 0:size], self.ones[:, :], esb[c][:, 0:size], start=True, stop=True),
                 reads=[("esb", c), "ones"], writes=[("ps", bz)])
            rz, tc_ = wk32[4 + c], wk32[6 + c]
            S.op("dve", lambda e: e.reciprocal(out=rz[:, 0:size], in_=ps[:, bz, 0:size]),
                 reads=[("ps", bz)], writes=[("rz", c)])
            S.op("dve", lambda e: e.tensor_tensor(out=tc_[:, 0:size], in0=ps[:, bo, 0:size], in1=rz[:, 0:size], op=ALU.mult),
                 reads=[("ps", bo), ("rz", c)], writes=[("tc", c)])
            if c == 0:
                return
            o_ = wk32[8]
            S.op("dve", lambda e: e.scalar_tensor_tensor(
                out=o_[:, 0:size], in0=wk32[7][:, 0:size], scalar=neglam, in1=wk32[6][:, 0:size], op0=ALU.mult, op1=ALU.add),
                reads=[("tc", 0), ("tc", 1), "neglam"], writes=["o_"])
            S.op("act", lambda e: e.activation(out=osq[:, 0:size], in_=o_[:, 0:size], func=AF.Square),
                 reads=["o_"], writes=["osq"])
            S.op("pe", lambda e: e.matmul(ps[:, 2, 0:size], self.ones[:, :], osq[:, 0:size], start=True, stop=True),
                 reads=["osq", "ones"], writes=[("ps", 2)])
            rs_, ltmp = wk32[9], wk32[5]
            self.rsqrt_act(rs_[:, 0:size], ps[:, 2, 0:size], 1.0 / 128.0, RMS_EPS, [("ps", 2)], "rs_", ltmp[:, 0:size], ("rz", 1))
            S.op("dve", lambda e: e.scalar_tensor_tensor(
                out=mix[:, 4 + hd, t0:t0 + size], in0=o_[:, 0:size], scalar=gsc, in1=rs_[:, 0:size], op0=ALU.mult, op1=ALU.mult),
                reads=["o_", "rs_", "gsc"], writes=[("mixa", hd, ti)])
        NI = len(iters)
        DPIPE = 2
        for n in range(min(DPIPE, NI)):
            front(n)
        for n in range(NI):
            if n + DPIPE < NI:
                front(n + DPIPE)
            back(n)
    self.dump("mixattn_b%d" % b, mix[:, 4:8, :], (128, 4, TT), BF16, [("mixa", hd, ti) for hd in range(4) for ti in range(5)])


K.s2_l0 = _s2_l0


def _s3(self, b, layer):
    S, T = self.S, self.T
    ps = self.psum
    self.phase_barrier()
    self.common_views()
    mix = self.mix
    wo = self.view(A_WB, (8, 1024), BF16)
    wname = "e_w_o" if layer == 0 else "o_w_o"
    S.dma("pool", lambda e: e.dma_start(out=wo[:, :, :], in_=T[wname].ap()[0].rearrange("(k p) n -> p k n", p=128)),
          writes=["wo"], semkey="wo")
    self.bc_load_ln(self.bc[1], ("bc", 1), "ln_g", layer, 0)
    self.bc_load_ln(self.bc[2], ("bc", 2), "ln_b", layer, 0)
    kinds = ("ctx", "lat") if layer == 0 else ("lat",)
    if layer == 1:
        wr32 = self.view(A_SCR, (8, 8), F32)
        wr32b = self.view(A_SCR + 256, (8, 8), F32)
        whi = self.view(A_SCR + 512, (8, 8), BF16)
        wlo = self.view(A_SCR + 640, (8, 8), BF16)
        lob = [self.view(A_SCR + 1024 + i * 2048, (1024,), BF16) for i in range(2)]
        lofm = [self.view(A_SCR + 1024 + 4096 + i * 2048, (8, 128), BF16) for i in range(2)]
        S.dma("sp", lambda e: e.dma_start(out=wr32[:, :, :], in_=T["o_router"].ap()[0].rearrange("(k p) e -> p k e", p=128)),
              writes=["wr32"], semkey="wr32")
        S.op("dve", lambda e: e.tensor_copy(out=whi[:, :, :], in_=wr32[:, :, :]), reads=["wr32"], writes=["whi"])
        S.op("dve", lambda e: e.tensor_tensor(out=wr32b[:, :, :], in0=wr32[:, :, :], in1=whi[:, :, :], op=ALU.subtract),
             reads=["wr32", "whi"], writes=["wr32b"])
        S.op("dve", lambda e: e.tensor_copy(out=wlo[:, :, :], in_=wr32b[:, :, :]), reads=["wr32b"], writes=["wlo"])
        wr_b = (whi, wlo, lob, lofm)
    self.tm_phase(b, layer, "s3", wr_b=(wr_b if layer == 1 else None), wo=wo)
    self.dump("xres_l%d_b%d" % (layer, b), T["xres"].ap()[b], (TT, D), F32, [("xres", b, t) for t in range(NT)])
    self.dump("hfm2_l%d_b%d" % (layer, b), self.hfm[:, :, :], (128, 8, TT), BF16, self.hfm_keys(0, TT))
    if layer == 1:
        self.dump("gates_b%d" % b, self.gates[:, :, :], (128, 16, 8), F32, [("gates", lt) for lt in range(16)])


K.s3 = _s3


def _router_tile(self, h32, hkey, wr_b, lt, junk, junkkey, r):
    S = self.S
    ps = self.psum
    whi, wlo, lob_, lofm_ = wr_b
    lob, lofm = lob_[r], lofm_[r]
    hb, khb = self.hb[r], ("hb", r)
    t = lt + 2
    lg = self.rts[:, r, 0:8]
    u = ("rt", r)
    S.op("dve", lambda e: e.tensor_tensor(out=lob, in0=h32, in1=hb, op=ALU.subtract), reads=[hkey, khb], writes=[("lob", r)])
    for half in range(2):
        bank = 4 + 2 * r + half

        def trl(e, half=half, bank=bank):
            for kk in range(4):
                k = half * 4 + kk
                ins = e.matmul(ps[:, bank, kk * 128:(kk + 1) * 128], lob[:, k * 128:(k + 1) * 128], self.ident[:, :], start=True, stop=True)
            return ins
        S.op("pe", trl, reads=[("lob", r), "ident"], writes=[("ps", bank)])
        S.op("act", lambda e, half=half, bank=bank: e.copy(out=lofm[:, half * 4:(half + 1) * 4, :],
                                                          in_=ps[:, bank, :].rearrange("p (a b) -> p a b", a=4)),
             reads=[("ps", bank)], writes=[("lofm", r, half)])
    lbank = 4 + 2 * r

    def mml(e):
        n_ = 0
        for k in range(8):
            for (a_, w_) in ((self.hfm[:, k, t * 128:(t + 1) * 128], whi), (self.hfm[:, k, t * 128:(t + 1) * 128], wlo), (lofm[:, k, :], whi)):
                ins = e.matmul(ps[:, lbank, 0:8], a_, w_[:, k, :], start=(n_ == 0), stop=(n_ == 23))
                n_ += 1
        return ins
    S.op("pe", mml, reads=[("hfm", t, 0), ("hfm", t, 1), ("lofm", r, 0), ("lofm", r, 1), "whi", "wlo"], writes=[("ps", lbank)])
    S.op("dve", lambda e: e.tensor_copy(out=lg, in_=ps[:, lbank, 0:8]), reads=[("ps", lbank)], writes=[(u, "lg", e_) for e_ in range(8)])
    lgk = [(u, "lg", e_) for e_ in range(8)]
    g = self.gates[:, lt, :]
    w8 = self.rts[:, r, 8:32].rearrange("p (a b) -> p a b", a=3)
    m1, m2, dd, ee, p1, p2 = (self.rts[:, r, 32 + i:33 + i] for i in range(6))
    S.op("dve", lambda e: e.reduce_max(out=m1, in_=lg, axis=AX.X), reads=lgk, writes=[(u, "m1")])
    S.op("dve", lambda e: e.tensor_scalar(out=w8[:, 0, :], in0=lg, scalar1=m1, scalar2=None, op0=ALU.is_equal),
         reads=lgk + [(u, "m1")], writes=[(u, "eq1")])
    S.op("dve", lambda e: e.scalar_tensor_tensor(out=w8[:, 1, :], in0=w8[:, 0, :], scalar=-1e30, in1=lg, op0=ALU.mult, op1=ALU.add),
         reads=lgk + [(u, "eq1")], writes=[(u, "l2")])
    S.op("dve", lambda e: e.reduce_max(out=m2, in_=w8[:, 1, :], axis=AX.X), reads=[(u, "l2")], writes=[(u, "m2")])
    S.op("dve", lambda e: e.tensor_scalar(out=w8[:, 2, :], in0=w8[:, 1, :], scalar1=m2, scalar2=None, op0=ALU.is_equal),
         reads=[(u, "l2"), (u, "m2")], writes=[(u, "eq2")])
    S.op("dve", lambda e: e.tensor_tensor(out=dd, in0=m2, in1=m1, op=ALU.subtract), reads=[(u, "m1"), (u, "m2")], writes=[(u, "dd")])
    S.op("act", lambda e: e.activation(out=ee, in_=dd, func=AF.Exp), reads=[(u, "dd")], writes=[(u, "ee")])
    S.op("dve", lambda e: e.tensor_scalar(out=p2, in0=ee, scalar1=1.0, scalar2=None, op0=ALU.add), reads=[(u, "ee")], writes=[(u, "den")])
    S.op("dve", lambda e: e.reciprocal(out=p1, in_=p2), reads=[(u, "den")], writes=[(u, "p1")])
    S.op("dve", lambda e: e.tensor_tensor(out=p2, in0=ee, in1=p1, op=ALU.mult), reads=[(u, "ee"), (u, "p1"), (u, "den")], writes=[(u, "p2")])
    S.op("dve", lambda e: e.tensor_scalar(out=w8[:, 0, :], in0=w8[:, 0, :], scalar1=p1, scalar2=None, op0=ALU.mult),
         reads=[(u, "eq1"), (u, "p1")], writes=[(u, "g1")])
    S.op("dve", lambda e: e.scalar_tensor_tensor(out=g, in0=w8[:, 2, :], scalar=p2, in1=w8[:, 0, :], op0=ALU.mult, op1=ALU.add),
         reads=[(u, "eq2"), (u, "p2"), (u, "g1")], writes=[("gates", lt)])


K.router_tile = _router_tile


def _swiglu(self, b, experts, tiles, first_tile):
    S = self.S
    ps = self.psum
    self.phase_barrier()
    self.common_views()
    acc = self.acc
    base = A_BC
    wsets = []
    for i in range(2):
        o = base + i * 24576
        wsets.append((self.view(o, (8, 512), BF16), self.view(o + 8192, (8, 512), BF16), self.view(o + 16384, (4, 1024), BF16)))
    hid = [self.view(base + 49152 + i * 4096, (4, 512), BF16) for i in range(2)]
    sg = [self.view(base + 49152 + 8192 + i * 1024, (512,), BF16) for i in range(2)]
    gi = 0
    first = True
    stages = []
    for (wg_ap, wu_ap, wd_ap, F, gcol) in experts:
        nch = F // 128
        c0 = 0
        while c0 < nch:
            ng = min(4, nch - c0)
            ws = gi % 2
            wg, wu, wd = wsets[ws]
            wgk, wuk, wdk = ("wg", ws), ("wu", ws), ("wd", ws)
            cols = slice(c0 * 128, (c0 + ng) * 128)
            S.capture()
            S.dma("pool", lambda e, wg=wg, wg_ap=wg_ap, cols=cols, ng=ng: e.dma_start(
                out=wg[:, :, 0:ng * 128], in_=wg_ap.rearrange("(k p) n -> p k n", p=128)[:, :, cols]),
                writes=[wgk], semkey=wgk)
            S.dma("pool", lambda e, wu=wu, wu_ap=wu_ap, cols=cols, ng=ng: e.dma_start(
                out=wu[:, :, 0:ng * 128], in_=wu_ap.rearrange("(k p) n -> p k n", p=128)[:, :, cols]),
                writes=[wuk], semkey=wuk)
            S.dma("pool", lambda e, wd=wd, wd_ap=wd_ap, c0=c0, ng=ng: e.dma_start(
                out=wd[:, 0:ng, :], in_=wd_ap[c0 * 128:(c0 + ng) * 128, :].rearrange("(c p) n -> p c n", p=128)),
                writes=[wdk], semkey=wdk)
            wlist = S.end_capture()
            for ti, (t0, size) in enumerate(tiles):
                hs = len(stages) % 2
                hd_, hk = hid[hs], ("hid", hs)
                S.capture()
                for c in range(ng):
                    pr = c % 2
                    bg_, bu_ = 0 + pr, 2 + pr

                    def mmg(e, c=c, bg_=bg_, bu_=bu_, t0=t0, size=size, wg=wg, wu=wu):
                        for k in range(8):
                            e.matmul(ps[:, bg_, 0:size], wg[:, k, c * 128:(c + 1) * 128], self.hfm[:, k, t0:t0 + size],
                                     start=(k == 0), stop=(k == 7))
                        for k in range(8):
                            ins = e.matmul(ps[:, bu_, 0:size], wu[:, k, c * 128:(c + 1) * 128], self.hfm[:, k, t0:t0 + size],
                                           start=(k == 0), stop=(k == 7))
                        return ins
                    S.op("pe", mmg, reads=[wgk, wuk] + self.hfm_keys(t0, size), writes=[("ps", bg_), ("ps", bu_)])
                    S.op("act", lambda e, pr=pr, bg_=bg_, size=size: e.activation(out=sg[pr][:, 0:size], in_=ps[:, bg_, 0:size], func=AF.Silu),
                         reads=[("ps", bg_)], writes=[("sg", pr)])
                    S.op("dve", lambda e, pr=pr, bu_=bu_, size=size, c=c, hd_=hd_: e.tensor_tensor(
                        out=hd_[:, c, 0:size], in0=ps[:, bu_, 0:size], in1=sg[pr][:, 0:size], op=ALU.mult),
                        reads=[("ps", bu_), ("sg", pr)], writes=[(hk, c)])
                glist = S.end_capture()
                S.capture()
                for s_ in range(size // 128):
                    tile_i = (t0 // 128) + s_ - first_tile
                    for n_ in range(2):
                        bank = 4 + (2 * s_ + n_) % 4

                        def mmd(e, s_=s_, n_=n_, bank=bank, hd_=hd_, wd=wd, ng=ng):
                            for c in range(ng):
                                ins = e.matmul(ps[:, bank, :], hd_[:, c, s_ * 128:(s_ + 1) * 128], wd[:, c, n_ * 512:(n_ + 1) * 512],
                                               start=(c == 0), stop=(c == ng - 1))
                            return ins
                        S.op("pe", mmd, reads=[wdk] + [(hk, c) for c in range(ng)], writes=[("ps", bank)])
                        a_ = acc[:, tile_i, n_ * 512:(n_ + 1) * 512]
                        ak = ("acc", tile_i, n_)
                        if gcol is None:
                            if first:
                                S.op("dve", lambda e, a_=a_, bank=bank: e.tensor_copy(out=a_, in_=ps[:, bank, :]),
                                     reads=[("ps", bank)], writes=[ak])
                            else:
                                S.op("dve", lambda e, a_=a_, bank=bank: e.tensor_tensor(out=a_, in0=ps[:, bank, :], in1=a_, op=ALU.add),
                                     reads=[("ps", bank), ak], writes=[ak])
                        else:
                            gsc_ = self.gates[:, tile_i, gcol:gcol + 1]
                            if first:
                                S.op("dve", lambda e, a_=a_, bank=bank, gsc_=gsc_: e.tensor_scalar(
                                    out=a_, in0=ps[:, bank, :], scalar1=gsc_, scalar2=None, op0=ALU.mult),
                                    reads=[("ps", bank), ("gates", tile_i)], writes=[ak])
                            else:
                                S.op("dve", lambda e, a_=a_, bank=bank, gsc_=gsc_: e.scalar_tensor_tensor(
                                    out=a_, in0=ps[:, bank, :], scalar=gsc_, in1=a_, op0=ALU.mult, op1=ALU.add),
                                    reads=[("ps", bank), ak, ("gates", tile_i)], writes=[ak])
                dlist = S.end_capture()
                stages.append((wlist if ti == 0 else None, glist, dlist))
            first = False
            gi += 1
            c0 += ng
    NS = len(stages)
    for n in range(NS):
        if n == 0:
            S.ops.extend(stages[0][0])
            S.ops.extend(stages[0][1])
        if n + 1 < NS:
            if stages[n + 1][0] is not None:
                S.ops.extend(stages[n + 1][0])
            S.ops.extend(stages[n + 1][1])
        S.ops.extend(stages[n][2])


K.swiglu = _swiglu


def _s4_l0(self, b):
    T = self.T
    self.swiglu(b, [(T["e_ffn_gate"].ap()[0], T["e_ffn_up"].ap()[0], T["e_ffn_down"].ap()[0], D_FF, None)],
                TOK_TILES, 0)


K.s4_l0 = _s4_l0


def _s4_l1(self, b):
    T = self.T
    ex = [(T["o_exp_gate"].ap()[0, e_], T["o_exp_up"].ap()[0, e_], T["o_exp_down"].ap()[0, e_], D_FFE, e_) for e_ in range(N_EXP)]
    self.swiglu(b, ex, TOK_TILES[1:], 2)


K.s4_l1 = _s4_l1


def _s5(self, b, layer):
    S, T = self.S, self.T
    self.phase_barrier()
    self.common_views()
    acc = self.acc
    self.bc_load_ln(self.bc[1], ("bc", 1), "ln_g", layer, 1)
    self.bc_load_ln(self.bc[2], ("bc", 2), "ln_b", layer, 1)
    self.tm_phase(b, layer, "s5")
    if layer == 0:
        self.dump("xres5_b%d" % b, T["xres"].ap()[b], (TT, D), F32, [("xres", b, t) for t in range(2, NT)])
        self.dump("hfm5_b%d" % b, self.hfm[:, :, :], (128, 8, TT), BF16, self.hfm_keys(0, TT))


K.s5 = _s5


def _layer0(self, b):
    self.s1(b)
    if self.stop == "l0s1":
        return
    self.s2_l0(b)
    if self.stop in ("l0conv", "l0s2"):
        return
    self.s3(b, 0)
    if self.stop == "l0s3":
        return
    self.s4_l0(b)
    self.s5(b, 0)


K.layer0 = _layer0


LAT_TILES = TOK_TILES[1:]


def _s2_l1(self, b):
    S, T = self.S, self.T
    ps = self.psum
    self.phase_barrier()
    self.common_views()
    mix = self.mix
    w_in = T["o_w_in"].ap()[0]
    B0 = A_SCR
    gfm = self.view(B0, (4, L), BF16)
    Atm = self.view(B0 + 16384, (16, 512), BF16)
    Btm = self.view(B0 + 32768, (16, 512), BF16)
    tabs = [(self.view(B0 + 49152 + i * 8192, (16, 128), BF16), self.view(B0 + 49152 + i * 8192 + 4096, (16, 128), BF16)) for i in range(2)]
    o2 = B0 + 49152 + 16384
    pcb = [self.view(o2 + i * 1024, (512,), BF16) for i in range(2)]
    sqb = [self.view(o2 + 2048 + i * 1024, (512,), BF16) for i in range(2)]
    ftm = [self.view(o2 + 4096 + i * 1024, (512,), BF16) for i in range(2)]
    f32t = [self.view(o2 + 6144 + i * 2048, (512,), F32) for i in range(8)]
    wst = [self.view(o2 + 6144 + 16384 + i * 2048, (8, 128), BF16) for i in range(4)]
    ccs = self.view(o2 + 6144 + 16384 + 8192, (2, 128), BF16)
    S.dma("sp", lambda e: e.dma_start(out=ccs[:, 0, :], in_=T["dft_cc"].ap()), writes=[("ccs", 0)], semkey=("ccs", 0))
    S.dma("sp", lambda e: e.dma_start(out=ccs[:, 1, :], in_=T["dft_sc"].ap()), writes=[("ccs", 1)], semkey=("ccs", 1))
    for g in range(4):
        self.load_wst(wst[g % 2], ("wst", g % 2), w_in, g * 128)
        for ti, (t0, size) in enumerate(LAT_TILES):
            r = ti % 2
            l0 = t0 - 256
            self.proj_fm(wst[g % 2], [("wst", g % 2)], t0, size, r)
            S.op("act", lambda e, r=r: e.copy(out=pcb[r], in_=ps[:, r, :]), reads=[("ps", r)], writes=[("pcb", r)])
            S.op("act", lambda e, r=r: e.activation(out=sqb[r], in_=ps[:, r, :], func=AF.Square), reads=[("ps", r)], writes=[("sqb", r)])
            S.op("pe", lambda e, r=r: e.matmul(ps[:, 2 + r, :], self.ones[:, :], pcb[r], start=True, stop=True),
                 reads=[("pcb", r), "ones"], writes=[("ps", 2 + r)])
            S.op("pe", lambda e, r=r: e.matmul(ps[:, 4 + r, :], self.ones[:, :], sqb[r], start=True, stop=True),
                 reads=[("sqb", r), "ones"], writes=[("ps", 4 + r)])
            mean, m2, var, ltmp, rstd, cen = (f32t[i] for i in (0 + r, 2 + r, 2 + r, 4 + r, 4 + r, 6 + r))
            S.op("dve", lambda e, r=r, mean=mean: e.tensor_scalar(out=mean, in0=ps[:, 2 + r, :], scalar1=1.0 / 128, scalar2=None, op0=ALU.mult),
                 reads=[("ps", 2 + r)], writes=[("f32t", 0 + r)])
            S.op("dve", lambda e, mean=mean, m2=m2: e.tensor_tensor(out=m2, in0=mean, in1=mean, op=ALU.mult),
                 reads=[("f32t", 0 + r)], writes=[("f32t", 2 + r)])
            S.op("dve", lambda e, r=r, m2=m2, var=var: e.scalar_tensor_tensor(out=var, in0=ps[:, 4 + r, :], scalar=1.0 / 128, in1=m2, op0=ALU.mult, op1=ALU.subtract),
                 reads=[("ps", 4 + r), ("f32t", 2 + r)], writes=[("f32t", 2 + r)])
            self.rsqrt_act(rstd, var, 1.0, LN_EPS, [("f32t", 2 + r)], ("f32t", 4 + r), ltmp, ("f32t", 4 + r))
            S.op("dve", lambda e, r=r, cen=cen, mean=mean: e.tensor_tensor(out=cen, in0=ps[:, r, :], in1=mean, op=ALU.subtract),
                 reads=[("ps", r), ("f32t", 0 + r)], writes=[("f32t", 6 + r)])
            S.op("dve", lambda e, cen=cen, rstd=rstd, g=g, l0=l0: e.tensor_tensor(out=gfm[:, g, l0:l0 + 512], in0=cen, in1=rstd, op=ALU.mult),
                 reads=[("f32t", 6 + r), ("f32t", 4 + r)], writes=[("gfm", g, ti)])
    gk = [("gfm", g, ti) for g in range(4) for ti in range(4)]
    self.dump("gfm_b%d" % b, gfm[:, :, :], (128, 4, L), BF16, gk)
    if self.stop == "l1fa":
        return
    for t in range(16):
        r = t % 2

        def mmab(e, t=t, r=r):
            for g in range(4):
                e.matmul(ps[:, r, g * 128:(g + 1) * 128], gfm[:, g, t * 128:(t + 1) * 128], ccs[:, 0, :], start=True, stop=True)
            for g in range(4):
                ins = e.matmul(ps[:, 2 + r, g * 128:(g + 1) * 128], gfm[:, g, t * 128:(t + 1) * 128], ccs[:, 1, :], start=True, stop=True)
            return ins
        S.op("pe", mmab, reads=gk + [("ccs", 0), ("ccs", 1)], writes=[("ps", r), ("ps", 2 + r)])
        S.op("act", lambda e, t=t, r=r: e.copy(out=Atm[:, t, :], in_=ps[:, r, :]), reads=[("ps", r)], writes=[("Atm", t)])
        S.op("dve", lambda e, t=t, r=r: e.tensor_copy(out=Btm[:, t, :], in_=ps[:, 2 + r, :]), reads=[("ps", 2 + r)], writes=[("Btm", t)])
    abk = [("Atm", t) for t in range(16)] + [("Btm", t) for t in range(16)]
    self.dump("atm_b%d" % b, Atm[:, :, :], (128, 16, 512), BF16, abk)
    if self.stop == "l1fb":
        return
    for mt in range(16):
        r = mt % 2
        cl, sl = tabs[r]
        S.dma("sp", lambda e, cl=cl, mt=mt: e.dma_start(out=cl[:, :, :], in_=T["dft_cl"].ap().rearrange("(c p) m -> p c m", p=128)[:, :, mt * 128:(mt + 1) * 128]),
              writes=[("cl", r)], semkey=("cl", r))
        S.dma("sp", lambda e, sl=sl, mt=mt: e.dma_start(out=sl[:, :, :], in_=T["dft_sl"].ap().rearrange("(c p) m -> p c m", p=128)[:, :, mt * 128:(mt + 1) * 128]),
              writes=[("sl", r)], semkey=("sl", r))

        def mmf(e, cl=cl, sl=sl, r=r):
            for c_ in range(16):
                e.matmul(ps[:, 4 + r, :], cl[:, c_, :], Atm[:, c_, :], start=(c_ == 0), stop=False)
            for c_ in range(16):
                ins = e.matmul(ps[:, 4 + r, :], sl[:, c_, :], Btm[:, c_, :], start=False, stop=(c_ == 15))
            return ins
        S.op("pe", mmf, reads=abk + [("cl", r), ("sl", r)], writes=[("ps", 4 + r)])
        S.op("act", lambda e, r=r: e.mul(out=ftm[r], in_=ps[:, 4 + r, :], mul=1.0 / 512),
             reads=[("ps", 4 + r)], writes=[("ftm", r)])

        def trf(e, r=r):
            for g in range(4):
                ins = e.matmul(ps[:, 6 + r, g * 128:(g + 1) * 128], ftm[r][:, g * 128:(g + 1) * 128], self.ident[:, :], start=True, stop=True)
            return ins
        S.op("pe", trf, reads=[("ftm", r), "ident"], writes=[("ps", 6 + r)])
        S.op("act", lambda e, r=r, mt=mt: e.copy(out=mix[:, 0:4, 256 + mt * 128: 256 + (mt + 1) * 128],
                                                in_=ps[:, 6 + r, :].rearrange("p (a b) -> p a b", a=4)),
             reads=[("ps", 6 + r)], writes=[("mixf", mt)])
    self.dump("mixfour_b%d" % b, mix[:, 0:4, :], (128, 4, TT), BF16, [("mixf", mt) for mt in range(16)])
    if self.stop == "l1four":
        return
    self.phase_barrier()
    qz = [self.view(B0, (4, L), BF16), self.view(B0 + 16384 + 2 * 18432 + 8192 + 15360 + 12288 + 4096 + 8192, (4, L), BF16)]
    S.op("dve", lambda e: e.memset(qz[0][64:128, :, :], 0.0), writes=[("qzz", 0)])
    S.op("dve", lambda e: e.memset(qz[1][0:64, :, :], 0.0), writes=[("qzz", 1)])
    kfm = self.view(B0 + 16384, (4, TT), BF16)
    vtm = self.view(B0 + 16384 + 18432, (NT, 512), BF16)
    o3 = B0 + 16384 + 2 * 18432
    EB = self.view(o3, (2, 8, 256), BF16)
    vtm2 = self.view(o3 + 8192, (15, 512), BF16)
    o4 = o3 + 8192 + 15360
    rev = [self.view(o4 + i * 1024, (256,), F32) for i in range(2)]
    ebt = [self.view(o4 + 2048 + i * 1024, (256,), F32) for i in range(2)]
    nav = self.view(o4 + 4096, (4, 64), F32)
    jrev = self.view(o4 + 5120, (128,), F32)
    zt = self.view(o4 + 5632, (640,), F32)
    Eb = [self.view(o4 + 8192 + i * 768, (384,), BF16) for i in range(4)]
    rzt = [self.view(o4 + 8192 + 3072 + i * 256, (64,), F32) for i in range(2)]
    wst = [self.view(o4 + 12288 + i * 2048, (8, 128), BF16) for i in range(2)]
    wv = self.view(o4 + 12288 + 4096, (8, 512), BF16)
    assert o4 + 12288 + 4096 + 8192 + 16384 <= self.ARENA_BYTES, o4
    S.op("dve", lambda e: e.memset(zt[0:8, :], 0.0), writes=["zt"])
    S.dma("sp", lambda e: e.dma_start(out=T["rpbp"].ap(), in_=zt[0:8, :]), reads=["zt"], writes=["rpbp"], semkey="rpbp0")
    S.dma("sp", lambda e: e.dma_start(out=T["rpbp"].ap()[:, 64:64 + 465], in_=T["rpbf"].ap()), reads=["rpbp"], writes=["rpbp"], semkey="rpbp1")
    for cb in range(4):
        S.dma("sp", lambda e, cb=cb: e.dma_start(out=nav[:, cb, :], in_=T["navalid"].ap()), writes=[("nav", cb)], semkey=("nav", cb))
    S.dma("sp", lambda e: e.dma_start(out=jrev, in_=T["jrev"].ap()), writes=["jrev"], semkey="jrev")
    navk = [("nav", cb) for cb in range(4)]
    def build_eb(h):
        for pi in range(8):
            r = (h * 8 + pi) % 2
            for hi in range(2):
                off = h * 640 + 64 + (pi + hi) * 31 - 48
                S.dma("sp", lambda e, r=r, hi=hi, off=off: e.dma_start(
                    out=rev[r][hi * 64:(hi + 1) * 64, :].rearrange("p (a b) -> p a b", a=4),
                    in_=flat_ap(T["rpbp"], off, [[1, 64], [62, 4], [1, 64]])),
                    reads=["rpbp"], writes=[("rev", r, hi)], semkey=("rev", r, hi))
            S.op("pe", lambda e, r=r: e.matmul(ps[:, 6 + r, 0:256], jrev, rev[r], start=True, stop=True),
                 reads=[("rev", r, 0), ("rev", r, 1), "jrev"], writes=[("ps", 6 + r)])
            S.op("act", lambda e, r=r: e.activation(out=ebt[r], in_=ps[:, 6 + r, 0:256], func=AF.Exp), reads=[("ps", 6 + r)], writes=[("ebt", r)])
            S.op("dve", lambda e, r=r, h=h, pi=pi: e.tensor_tensor(out=EB[:, h % 2, pi, :], in0=ebt[r], in1=nav[:, :, :].rearrange("p a b -> p (a b)"), op=ALU.mult),
                 reads=[("ebt", r)] + navk, writes=[("EB", h % 2, pi)])
    for j in range(4):
        self.load_wst(wst[j % 2], ("wst", j % 2), w_in, 512 + j * 128)
        for ti, (t0, size) in enumerate(LAT_TILES):
            r = ti % 2
            self.proj_fm(wst[j % 2], [("wst", j % 2)], t0, size, 2 + r)
            for c_ in range(2):
                S.op("act", lambda e, r=r, j=j, t0=t0, c_=c_: e.copy(out=qz[c_][c_ * 64:(c_ + 1) * 64, j, t0 - 256:t0 - 256 + 512],
                                                                  in_=ps[c_ * 64:(c_ + 1) * 64, 2 + r, :]),
                     reads=[("ps", 2 + r)], writes=[("qfm", j, ti, c_)])
    for j in range(4):
        self.load_wst(wst[j % 2], ("wst", j % 2), w_in, 1024 + j * 128)
        for ti, (t0, size) in enumerate(TOK_TILES):
            r = ti % 2
            self.proj_fm(wst[j % 2], [("wst", j % 2)], t0, size, 2 + r)
            S.op("act", lambda e, r=r, j=j, t0=t0, size=size: e.copy(out=kfm[:, j, t0:t0 + size], in_=ps[:, 2 + r, 0:size]),
                 reads=[("ps", 2 + r)], writes=[("kfm", j, ti)])
    self.load_wst(wv, "wv", w_in, 1536, ncols=512)
    for t in range(NT):
        bank = 4 + t % 2

        def mmv(e, t=t, bank=bank):
            for k in range(8):
                ins = e.matmul(ps[:, bank, :], self.hfm[:, k, t * 128:(t + 1) * 128], wv[:, k, :], start=(k == 0), stop=(k == 7))
            return ins
        S.op("pe", mmv, reads=["wv"] + self.hfm_keys(t * 128, 128), writes=[("ps", bank)])
        S.op("act", lambda e, t=t, bank=bank: e.copy(out=vtm[:, t, :], in_=ps[:, bank, :]), reads=[("ps", bank)], writes=[("vtm", t)])
    for t in range(15):
        bank = 4 + t % 2

        def mmv2(e, t=t, bank=bank):
            for k in range(8):
                ins = e.matmul(ps[:, bank, :], self.hfm[:, k, 320 + t * 128:320 + (t + 1) * 128], wv[:, k, :], start=(k == 0), stop=(k == 7))
            return ins
        S.op("pe", mmv2, reads=["wv"] + self.hfm_keys(320 + t * 128, 128), writes=[("ps", bank)])
        S.op("act", lambda e, t=t, bank=bank: e.copy(out=vtm2[:, t, :], in_=ps[:, bank, :]), reads=[("ps", bank)], writes=[("vtm2", t)])
    rows = [(j, c, r_) for j in range(4) for c in range(2) for r_ in range(32)]
    sbanks = (0, 1, 4)

    def kblocks(r_):
        rs = min(max(r_ - 4, 0), 24)
        kb = []
        for cb in range(4):
            row0 = rs + 2 * (3 - cb)
            if row0 % 2 == 0:
                kb.append((256 + row0 * 64, vtm[:, 2 + row0 // 2, :], ("vtm", 2 + row0 // 2)))
            else:
                kb.append((256 + row0 * 64, vtm2[:, (row0 - 1) // 2, :], ("vtm2", (row0 - 1) // 2)))
        kb += [(0, vtm[:, 0, :], ("vtm", 0)), (128, vtm[:, 1, :], ("vtm", 1))]
        return kb, r_ - rs

    def nfront(n):
        j, c, r_ = rows[n]
        if r_ == 0:
            build_eb(2 * j + c)
        sb = sbanks[n % 3]
        pl, ph = c * 64, (c + 1) * 64
        kb, _ = kblocks(r_)

        def mms(e):
            for blk, (kc0, _, _) in enumerate(kb):
                ins = e.matmul(ps[:, sb, blk * 64:(blk + 1) * 64], kfm[:, j, kc0:kc0 + 128],
                               qz[c][:, j, r_ * 64:(r_ + 1) * 64], start=True, stop=True)
            return ins
        S.op("pe", mms, reads=[("qfm", j, ti, c) for ti in range(4)] + [("qzz", c)] + [("kfm", j, ti) for ti in range(5)], writes=[("ps", sb)])

    def stageB(n):
        j, c, r_ = rows[n]
        h = 2 * j + c
        sb = sbanks[n % 3]
        es = n % 4
        kb, pi = kblocks(r_)
        S.op("act", lambda e: e.activation(out=Eb[es], in_=ps[:, sb, 0:384], func=AF.Exp, scale=0.125),
             reads=[("ps", sb)], writes=[("Eb", es)])
        S.op("dve", lambda e: e.tensor_tensor(out=Eb[es][:, 0:256], in0=Eb[es][:, 0:256], in1=EB[:, h % 2, pi, :], op=ALU.mult),
             reads=[("Eb", es), ("EB", h % 2, pi)], writes=[("Eb", es)])

    def stageC(n):
        j, c, r_ = rows[n]
        ob = (2, 3, 5)[n % 3]
        es = n % 4
        kb, pi = kblocks(r_)

        def mmo(e):
            for blk, (_, vt, _) in enumerate(kb):
                e.matmul(ps[:, ob, 0:64], vt[:, j * 128:(j + 1) * 128], Eb[es][:, blk * 64:(blk + 1) * 64],
                         start=(blk == 0), stop=(blk == 5))
            for blk in range(6):
                ins = e.matmul(ps[:, ob, 64:128], self.ones[:, :], Eb[es][:, blk * 64:(blk + 1) * 64],
                               start=(blk == 0), stop=(blk == 5))
            return ins
        S.op("pe", mmo, reads=[("Eb", es), "ones"] + [vk for (_, _, vk) in kb], writes=[("ps", ob)])

    def stageD(n):
        j, c, r_ = rows[n]
        h = 2 * j + c
        ob = (2, 3, 5)[n % 3]
        pl, ph = c * 64, (c + 1) * 64
        rz = rzt[n % 2]
        S.op("dve", lambda e: e.reciprocal(out=rz[pl:ph, :], in_=ps[pl:ph, ob, 64:128]),
             reads=[("ps", ob)], writes=[("rzt", n % 2)])
        S.op("dve", lambda e: e.tensor_tensor(
            out=mix[pl:ph, 4 + j, 256 + r_ * 64:256 + (r_ + 1) * 64], in0=ps[pl:ph, ob, 0:64], in1=rz[pl:ph, :], op=ALU.mult),
            reads=[("ps", ob), ("rzt", n % 2)], writes=[("mixn", h, r_)])
    NR = len(rows)
    for step in range(NR + 3):
        if step < NR:
            nfront(step)
        if 0 <= step - 1 < NR:
            stageB(step - 1)
        if 0 <= step - 2 < NR:
            stageC(step - 2)
        if 0 <= step - 3 < NR:
            stageD(step - 3)
    self.dump("mixna_b%d" % b, mix[:, 4:8, :], (128, 4, TT), BF16, [("mixn", h, r_) for h in range(8) for r_ in range(32)])


K.s2_l1 = _s2_l1


def _layer1(self, b):
    self.s2_l1(b)
    if self.stop in ("l1four", "l1s2", "l1fa", "l1fb"):
        return
    self.s3(b, 1)
    if self.stop == "l1s3":
        return
    self.s4_l1(b)
    self.s5(b, 1)


K.layer1 = _layer1


def _core_inputs(inputs, b0, NB, hc):
    m = {}
    m["x"] = np.ascontiguousarray(inputs["x"][b0:b0 + NB])
    m["c"] = np.ascontiguousarray(inputs["c"][b0:b0 + NB])
    m["ctx"] = np.ascontiguousarray(inputs["ctx"][b0:b0 + NB])
    for k in W_SHAPES:
        m[k] = np.ascontiguousarray(np.asarray(inputs[k], dtype=np.float32))
    rp = np.asarray(inputs["o_rpb"], dtype=np.float32)[0]
    m["rpbf"] = np.ascontiguousarray(rp[:, ::-1, ::-1]).reshape(8, 15 * 31)
    m["routerT"] = np.ascontiguousarray(np.asarray(inputs["o_router"], dtype=np.float32)[0].T)
    m.update(hc)
    return m


def run(inputs, NB=2, ncores=8, stop=None, dumps=(), trace=False):
    kb = K(NB=NB, stop=stop, dumps=dumps)
    nc = kb.build()
    hc = host_consts()
    in_maps = [_core_inputs(inputs, i * NB, NB, hc) for i in range(ncores)]
    res = run_bass_kernel_spmd(nc, in_maps, core_ids=list(range(ncores)), trace=trace)
    return kb, res


def kernel(**inputs):
    inputs = {k: np.asarray(v) for k, v in inputs.items()}
    kb, res = run(inputs, NB=2, ncores=8)
    out = np.concatenate([np.asarray(r["out"]) for r in res.results], axis=0)
    return out.astype(np.float32)
```

```python
import contextlib
import math
import numpy as np
import ml_dtypes
import concourse.bass as bass
import concourse.mybir as mybir
from concourse.ap import AP
from concourse.bass_utils import run_bass_kernel_spmd

F32 = mybir.dt.float32
BF16 = mybir.dt.bfloat16
AF = mybir.ActivationFunctionType
ALU = mybir.AluOpType
AX = mybir.AxisListType

D = 1024
L = 2048
LC = 256
TT = L + LC
NT = TT // 128
DEPTH = 2
ALPHA = (2 * DEPTH) ** 0.25
LN_EPS = 1e-5
RMS_EPS = 1e-5
LAM_INIT0 = 0.8 - 0.6 * math.exp(-0.3 * 0)
D_FF = 2816
N_EXP = 8
D_FFE = 3584
GRID_W = 64


class Sched:
    ENGS = ("pe", "act", "dve", "pool", "sp")

    def __init__(self, nc):
        self.nc = nc
        self.ops = []

    def op(self, eng, fn, reads=(), writes=()):
        self.ops.append(dict(eng=eng, fn=fn, reads=tuple(reads), writes=tuple(writes),
                             dma=False, semkey=None, barrier=False))

    def dma(self, queue, fn, reads=(), writes=(), semkey=None):
        assert semkey is not None
        self.ops.append(dict(eng=queue, fn=fn, reads=tuple(reads), writes=tuple(writes),
                             dma=True, semkey=semkey, barrier=False))

    def capture(self):
        self._saved = self.ops
        self.ops = []

    def end_capture(self):
        c = self.ops
        self.ops = self._saved
        return c

    def extend_interleaved(self, lists):
        n = max(len(l) for l in lists)
        for i in range(n):
            for l in lists:
                if i < len(l):
                    self.ops.append(l[i])

    def barrier(self, fn):
        self.ops.append(dict(eng="dve", fn=fn, reads=(), writes=(), dma=False, semkey=None,
                             barrier=True))

    def run(self):
        nc = self.nc
        ops = self.ops
        n = len(ops)
        last_w = {}
        readers = {}
        deps = [None] * n
        last_on_eng = {}
        dma_since_barrier = []
        last_barrier = None
        for i, o in enumerate(ops):
            if o["barrier"]:
                keep = set(last_on_eng.values()) | set(dma_since_barrier)
                if last_barrier is not None:
                    keep.add(last_barrier)
                deps[i] = keep
                last_barrier = i
                dma_since_barrier = []
                last_on_eng = {}
                last_w = {}
                readers = {}
                continue
            d = set()
            rd = list(o["reads"])
            wr = list(o["writes"])
            ps_r = [r for r in rd if isinstance(r, tuple) and r and r[0] == "ps"]
            rd = [r for r in rd if r not in ps_r]
            wr = wr + ps_r
            for r in rd:
                if r in last_w:
                    d.add((last_w[r], "raw"))
            for w in wr:
                if w in last_w:
                    d.add((last_w[w], "waw"))
                for q in readers.get(w, ()):
                    d.add((q, "war"))
            for r in rd:
                readers.setdefault(r, []).append(i)
            for w in wr:
                last_w[w] = i
                readers[w] = []
            keep = set()
            for (j, kind) in d:
                if j == i:
                    continue
                pj = ops[j]
                if pj["dma"] or o["dma"]:
                    keep.add(j)
                    continue
                if pj["eng"] == o["eng"]:
                    if o["eng"] == "pe":
                        continue
                    keep.add(j)
                    continue
                keep.add(j)
            if last_barrier is not None:
                keep.add(last_barrier)
            deps[i] = keep
            if o["dma"]:
                dma_since_barrier.append(i)
            else:
                last_on_eng[o["eng"]] = i
        needed = [False] * n
        for i in range(n):
            for j in deps[i]:
                needed[j] = True
        sig = [None] * n
        cnt = {}
        for i, o in enumerate(ops):
            if o["dma"]:
                key = ("dma", o["semkey"])
                cnt[key] = cnt.get(key, 0) + 16
                sig[i] = (key, cnt[key], 16)
            elif needed[i]:
                key = ("eng", o["eng"])
                cnt[key] = cnt.get(key, 0) + 1
                sig[i] = (key, cnt[key], 1)
        streams = {e: [] for e in self.ENGS}
        waited = {e: {} for e in self.ENGS}
        issued = {}
        for i, o in enumerate(ops):
            e = o["eng"]
            w = {}
            for j in deps[i]:
                key, val, _ = sig[j]
                if val > w.get(key, 0):
                    w[key] = val
            for key, val in w.items():
                if key[0] == "dma" and issued.get(key, 0) > val and not (o["dma"] and ("dma", o["semkey"]) == key):
                    print("SCHED WARNING: partial DMA-sem wait", key, val, issued[key], "op", i, o["eng"])
            if o["dma"]:
                issued[("dma", o["semkey"])] = sig[i][1]
            wl = []
            for key, val in w.items():
                if waited[e].get(key, 0) >= val:
                    continue
                waited[e][key] = val
                wl.append((key, val))
            streams[e].append((i, wl))
        with contextlib.ExitStack() as st:
            sems = {}
            for s in sig:
                if s is not None and s[0] not in sems:
                    sems[s[0]] = st.enter_context(nc.semaphore("s%d" % len(sems)))
            self.n_sems = len(sems)
            block = st.enter_context(nc.Block())

            def make(engname):
                def body(eng):
                    for (i, wl) in streams[engname]:
                        for key, val in wl:
                            eng.wait_ge(sems[key], val)
                        inst = ops[i]["fn"](eng)
                        s = sig[i]
                        if s is not None:
                            assert inst is not None, "op %d returned no instruction" % i
                            inst.then_inc(sems[s[0]], s[2])
                return body

            block.tensor(make("pe"))
            block.scalar(make("act"))
            block.vector(make("dve"))
            block.gpsimd(make("pool"))
            block.sync(make("sp"))


def host_consts():
    c = {}
    c["ident"] = np.eye(128, dtype=np.float32).astype(ml_dtypes.bfloat16)
    c["ones"] = np.ones((128, 128), dtype=np.float32).astype(ml_dtypes.bfloat16)
    c["jrev"] = np.eye(128, dtype=np.float32)[::-1].copy()
    t = np.arange(L, dtype=np.int32)
    row = (t // GRID_W).astype(np.float32)
    col = (t % GRID_W).astype(np.float32)
    nf = 16
    inv = (np.float32(10000.0) ** (-np.arange(nf, dtype=np.float32) / np.float32(nf))).astype(np.float32)
    ang = np.concatenate([row[:, None] * inv, col[:, None] * inv], axis=-1)
    cos = np.cos(ang).astype(np.float32).T
    sin = np.sin(ang).astype(np.float32).T
    cosf = np.concatenate([cos, cos, cos, cos], axis=0)
    sinf = np.concatenate([-sin, sin, -sin, sin], axis=0)
    c["ropec"] = cosf.astype(ml_dtypes.bfloat16)
    c["ropes"] = sinf.astype(ml_dtypes.bfloat16)
    n = np.arange(L, dtype=np.int64)
    ph = (np.outer(n, n) % L).astype(np.float64) * (2 * np.pi / L)
    c["dft_cl"] = np.cos(ph).astype(np.float32).astype(ml_dtypes.bfloat16)
    c["dft_sl"] = (-np.sin(ph)).astype(np.float32).astype(ml_dtypes.bfloat16)
    m = np.arange(128, dtype=np.int64)
    ph2 = (np.outer(m, m) % 128).astype(np.float64) * (2 * np.pi / 128)
    c["dft_cc"] = np.cos(ph2).astype(np.float32).astype(ml_dtypes.bfloat16)
    c["dft_sc"] = np.sin(ph2).astype(np.float32).astype(ml_dtypes.bfloat16)
    qc = np.arange(64)
    cs = np.clip(qc - 8, 0, 48)
    kc = np.arange(64)
    valid = ((kc[:, None] >= cs[None, :]) & (kc[:, None] < cs[None, :] + 16)).astype(np.float32)
    c["navalid"] = np.concatenate([valid, valid], axis=0)
    return c


CONST_DT = dict(ident=BF16, ones=BF16, jrev=F32, ropec=BF16, ropes=BF16, dft_cl=BF16, dft_sl=BF16,
                dft_cc=BF16, dft_sc=BF16, navalid=F32)

W_SHAPES = dict(
    w_mod=(2, 1024, 6144), b_mod=(2, 6144), ln_g=(2, 2, 1024), ln_b=(2, 2, 1024),
    e_w_in=(1, 1024, 3072), e_conv=(1, 3, 512), e_lam_q1=(1, 64), e_lam_k1=(1, 64),
    e_lam_q2=(1, 64), e_lam_k2=(1, 64), e_subln_g=(1, 128), e_w_o=(1, 1024, 1024),
    e_ffn_gate=(1, 1024, 2816), e_ffn_up=(1, 1024, 2816), e_ffn_down=(1, 2816, 1024),
    o_w_in=(1, 1024, 2048), o_w_o=(1, 1024, 1024),
    o_exp_gate=(1, 8, 1024, 3584), o_exp_up=(1, 8, 1024, 3584), o_exp_down=(1, 8, 3584, 1024),
    c_ctx=(1024,), o_router=(1, 1024, 8),
)


def flat_ap(t, offset, dims):
    return AP(t, offset, [list(d) for d in dims])


class K:
    def __init__(self, NB=2, stop=None, dumps=()):
        self.NB = NB
        self.R = NB + 1
        self.stop = stop
        self.dumps = set(dumps)
        self.dump_specs = {}
        self.nc = nc = bass.Bass("TRN2", target_bir_lowering=False)
        self.S = Sched(nc)
        self.T = {}
        T = self.T
        T["x"] = nc.dram_tensor("x", [NB, L, D], F32, kind="ExternalInput")
        T["c"] = nc.dram_tensor("c", [NB, D], F32, kind="ExternalInput")
        T["ctx"] = nc.dram_tensor("ctx", [NB, LC, D], F32, kind="ExternalInput")
        for k, shp in W_SHAPES.items():
            T[k] = nc.dram_tensor(k, list(shp), F32, kind="ExternalInput")
        T["rpbf"] = nc.dram_tensor("rpbf", [8, 15 * 31], F32, kind="ExternalInput")
        T["routerT"] = nc.dram_tensor("routerT", [8, 1024], F32, kind="ExternalInput")
        hc = host_consts()
        for k, v in hc.items():
            T[k] = nc.dram_tensor(k, list(v.shape), CONST_DT[k], kind="ExternalInput")
        T["out"] = nc.dram_tensor("out", [NB, L, D], F32, kind="ExternalOutput")
        T["modv"] = nc.dram_tensor("modv", [2, self.R, 6144], F32, kind="Internal")
        T["xres"] = nc.dram_tensor("xres", [NB, TT, D], F32, kind="Internal")
        T["lamd"] = nc.dram_tensor("lamd", [1, 8], F32, kind="Internal")
        T["rpbp"] = nc.dram_tensor("rpbp", [8, 640], F32, kind="Internal")
        self._bank_rr = {}
        self._uid = 0

    def uid(self):
        self._uid += 1
        return self._uid

    def view(self, off, shape, dt):
        esz = 4 if dt == F32 else 2
        nel = int(np.prod(shape))
        nb = nel * esz
        assert off % 4 == 0 and off + nb <= self.ARENA_BYTES, (off, nb, self.ARENA_BYTES)
        a = self.arena[:, off // 2: (off + nb) // 2]
        if dt == F32:
            a = a.bitcast(F32)
        if len(shape) == 2:
            a = a.rearrange("p (a b) -> p a b", a=shape[0])
        elif len(shape) == 3:
            a = a.rearrange("p (a b c) -> p a b c", a=shape[0], b=shape[1])
        return a

    def dump(self, name, ap, shape, dt, reads):
        if name not in self.dumps:
            return
        t = self.nc.dram_tensor("dbg_" + name, list(shape), dt, kind="ExternalOutput")
        self.dump_specs[name] = (shape, dt)
        self.S.dma("sp", lambda e, t=t, ap=ap: e.dma_start(out=t.ap(), in_=ap), reads=reads,
                   writes=[("dbg", name)], semkey=("dbg", name))
        self.final_reads.append(("dbg", name))

    def phase_barrier(self):
        bt = self.bar_tile
        self.S.barrier(lambda e: e.memset(bt[:, 0:1], 0.0))

    def build(self):
        nc = self.nc
        S = self.S
        self.final_reads = []
        with contextlib.ExitStack() as st:
            self.hfm = st.enter_context(nc.sbuf_tensor("hfm", [128, 8, TT], BF16))
            self.ARENA_BYTES = 156 * 1024
            self.arena = st.enter_context(nc.sbuf_tensor("arena", [128, self.ARENA_BYTES // 2], BF16))
            self.psum = st.enter_context(nc.psum_tensor("psum", [128, 8, 512], F32))
            self.ident = st.enter_context(nc.sbuf_tensor("ident_sb", [128, 128], BF16))
            self.ones = st.enter_context(nc.sbuf_tensor("ones_sb", [128, 128], BF16))
            self.bar_tile = st.enter_context(nc.sbuf_tensor("bar", [128, 4], F32))
            self.small = st.enter_context(nc.sbuf_tensor("small", [128, 64], F32))
            self.gates = st.enter_context(nc.sbuf_tensor("gates", [128, 16, 8], F32))
            self.gw8 = st.enter_context(nc.sbuf_tensor("gw8", [128, 3, 8], F32))
            self.gsm = st.enter_context(nc.sbuf_tensor("gsm", [128, 8], F32))
            self.rts = st.enter_context(nc.sbuf_tensor("rts", [128, 2, 48], F32))
            self.nmr = st.enter_context(nc.sbuf_tensor("nmr", [128, 2], F32))
            T = self.T
            S.dma("sp", lambda e: e.dma_start(out=self.ident[:], in_=T["ident"].ap()), writes=["ident"], semkey="c_ident")
            S.dma("sp", lambda e: e.dma_start(out=self.ones[:], in_=T["ones"].ap()), writes=["ones"], semkey="c_ones")
            self.phase_mod()
            for b in range(self.NB):
                if self.stop == "mod":
                    break
                self.layer0(b)
                if self.stop and self.stop.startswith("l0"):
                    continue
                self.layer1(b)
            self.phase_barrier()
            S.op("sp", lambda e: e.nop(), reads=self.final_reads)
            S.run()
        return nc

    def phase_mod(self):
        S, T, R, NB = self.S, self.T, self.R, self.NB
        self.phase_barrier()
        cT = self.view(0, (8, R), F32)
        scT = self.view(256, (8, R), F32)
        NWM = 4
        wm = [self.view(1024 + i * 16384, (8, 512), F32) for i in range(NWM)]
        modsb = self.view(1024 + NWM * 16384, (6144,), F32)
        bmod = self.view(1024 + NWM * 16384 + 24576, (6144,), F32)
        for r in range(R):
            if r < NB:
                src = flat_ap(T["c"], r * D, [[1, 128], [128, 8], [1, 1]])
            else:
                src = flat_ap(T["c_ctx"], 0, [[1, 128], [128, 8], [1, 1]])
            S.dma("sp", lambda e, src=src, r=r: e.dma_start(out=cT[:, :, r:r + 1], in_=src,
                                                          allow_slow_non_contiguous=True),
                  writes=[("cT", r)], semkey="cT")
        S.op("act", lambda e: e.activation(out=scT[:, :, :], in_=cT[:, :, :], func=AF.Silu),
             reads=[("cT", r) for r in range(R)], writes=["scT"])
        ps = self.psum
        for i in range(2):
            S.dma("sp", lambda e, i=i: e.dma_start(out=bmod[0:R, :], in_=flat_ap(T["b_mod"], i * 6144, [[0, R], [1, 6144]])),
                  writes=["bmod"], semkey="bmod")
            for ng in range(12):
                w = wm[ng % NWM]
                wsrc = T["w_mod"].ap()[i].rearrange("(k p) n -> p k n", p=128)[:, :, ng * 512:(ng + 1) * 512]
                S.dma("sp", lambda e, w=w, wsrc=wsrc: e.dma_start(out=w[:, :, :], in_=wsrc),
                      writes=[("wm", ng % NWM)], semkey=("wm", ng % NWM))
                bank = ng % 2

                def mm(e, w=w, bank=bank):
                    for k in range(8):
                        ins = e.matmul(ps[0:R, bank, :], scT[:, k, :], w[:, k, :], start=(k == 0), stop=(k == 7))
                    return ins
                S.op("pe", mm, reads=["scT", ("wm", ng % NWM)], writes=[("ps", bank)])
                S.op("dve", lambda e, bank=bank, ng=ng: e.tensor_tensor(
                    out=modsb[0:R, ng * 512:(ng + 1) * 512], in0=ps[0:R, bank, :],
                    in1=bmod[0:R, ng * 512:(ng + 1) * 512], op=ALU.add),
                    reads=[("ps", bank), "bmod"], writes=[("modsb", ng)])
            for j in (1, 4):
                S.op("dve", lambda e, j=j: e.tensor_scalar(out=modsb[0:R, j * 1024:(j + 1) * 1024],
                                                           in0=modsb[0:R, j * 1024:(j + 1) * 1024],
                                                           scalar1=1.0, scalar2=None, op0=ALU.add),
                     reads=[("modsb", 2 * j), ("modsb", 2 * j + 1)], writes=[("modsb", 2 * j), ("modsb", 2 * j + 1)])
            S.dma("sp", lambda e, i=i: e.dma_start(out=T["modv"].ap()[i], in_=modsb[0:R, :]),
                  reads=[("modsb", g) for g in range(12)], writes=[("modv", i)], semkey="modv")
        self.dump("modsb", modsb[0:R, :], (R, 6144), F32, [("modv", 1)])
        sm = self.small
        lq = self.view(1024, (4, 64), F32)
        for n_, nm in enumerate(("e_lam_q1", "e_lam_k1", "e_lam_q2", "e_lam_k2")):
            S.dma("sp", lambda e, n_=n_, nm=nm: e.dma_start(out=lq[0:1, n_, :], in_=T[nm].ap()),
                  writes=[("lq", n_)], semkey=("lq", n_))
        junk = self.view(2048, (64,), F32)
        for h_ in range(2):
            S.op("dve", lambda e, h_=h_: e.tensor_tensor(out=junk[0:1, :], in0=lq[0:1, 2 * h_, :], in1=lq[0:1, 2 * h_ + 1, :], op=ALU.mult),
                 reads=[("lq", 2 * h_), ("lq", 2 * h_ + 1)], writes=["junk"])
            S.op("dve", lambda e, h_=h_: e.reduce_sum(out=sm[0:1, h_:h_ + 1], in_=junk[0:1, :], axis=AX.X),
                 reads=["junk"], writes=[("sm", h_)])
        S.op("act", lambda e: e.activation(out=sm[0:1, 2:4], in_=sm[0:1, 0:2], func=AF.Exp),
             reads=[("sm", 0), ("sm", 1)], writes=[("sm", 2)])
        S.op("dve", lambda e: e.tensor_tensor(out=sm[0:1, 4:5], in0=sm[0:1, 3:4], in1=sm[0:1, 2:3], op=ALU.subtract),
             reads=[("sm", 2)], writes=[("sm", 4)])
        S.op("dve", lambda e: e.tensor_scalar(out=sm[0:1, 5:6], in0=sm[0:1, 4:5], scalar1=-LAM_INIT0, scalar2=None, op0=ALU.add),
             reads=[("sm", 4)], writes=[("sm", 5)])
        S.dma("sp", lambda e: e.dma_start(out=T["lamd"].ap()[0:1, 0:1], in_=sm[0:1, 5:6]),
              reads=[("sm", 5)], writes=["lamd"], semkey="lamd")
        S.dma("sp", lambda e: e.dma_start(out=sm[:, 8:9], in_=flat_ap(T["lamd"], 0, [[0, 128], [1, 1]])),
              reads=["lamd"], writes=["neglam"], semkey="neglam")
        S.dma("sp", lambda e: e.dma_start(out=sm[:, 10:11], in_=flat_ap(T["e_subln_g"], 0, [[1, 128], [1, 1]])),
              writes=["gsc0"], semkey="gsc0")
        S.op("dve", lambda e: e.tensor_scalar(out=sm[:, 9:10], in0=sm[:, 10:11], scalar1=1.0 - LAM_INIT0, scalar2=None, op0=ALU.mult),
             reads=["gsc0"], writes=["gsc"])
        self.dump("small", sm[:, :], (128, 64), F32, ["gsc", "neglam"])

    def bc_load(self, slot_ap, key, layer, row, j):
        T = self.T
        src = flat_ap(T["modv"], (layer * self.R + row) * 6144 + j * 1024, [[0, 128], [1, 1024]])
        self.S.dma("sp", lambda e: e.dma_start(out=slot_ap, in_=src), reads=[("modv", layer)],
                   writes=[key], semkey=key)

    def bc_load_ln(self, slot_ap, key, name, layer, s):
        T = self.T
        src = flat_ap(T[name], (layer * 2 + s) * 1024, [[0, 128], [1, 1024]])
        self.S.dma("sp", lambda e: e.dma_start(out=slot_ap, in_=src), writes=[key], semkey=key)

    def modulate_transpose(self, xt, xkey, sc, sckey, sh, shkey, tmp, tmpkey, hb, hbkey, t, banks, h32=None, h32key=None):
        S = self.S
        ps = self.psum
        S.op("dve", lambda e: e.tensor_tensor(out=tmp, in0=xt, in1=sc, op=ALU.mult),
             reads=[xkey, sckey], writes=[tmpkey])
        if h32 is None:
            S.op("dve", lambda e: e.tensor_tensor(out=hb, in0=tmp, in1=sh, op=ALU.add),
                 reads=[tmpkey, shkey], writes=[hbkey])
        else:
            S.op("dve", lambda e: e.tensor_tensor(out=h32, in0=tmp, in1=sh, op=ALU.add),
                 reads=[tmpkey, shkey], writes=[h32key])
            S.op("act", lambda e: e.copy(out=hb, in_=h32), reads=[h32key], writes=[hbkey])
        for half in range(2):
            bank = banks[half]

            def tr(e, half=half, bank=bank):
                for kk in range(4):
                    k = half * 4 + kk
                    ins = e.matmul(ps[:, bank, kk * 128:(kk + 1) * 128], hb[:, k * 128:(k + 1) * 128],
                                   self.ident[:, :], start=True, stop=True)
                return ins
            S.op("pe", tr, reads=[hbkey, "ident"], writes=[("ps", bank)])
            S.op("act", lambda e, half=half, bank=bank: e.copy(
                out=self.hfm[:, half * 4:(half + 1) * 4, t * 128:(t + 1) * 128],
                in_=ps[:, bank, :].rearrange("p (a b) -> p a b", a=4)),
                reads=[("ps", bank)], writes=[("hfm", t, half)])

    def hfm_keys(self, t0, size):
        return [("hfm", t, h) for t in range(t0 // 128, (t0 + size + 127) // 128) for h in range(2)]

    def layer_norm_tile(self, tt, ttkey, lng, lngkey, lnb, lnbkey, xo, xokey, stats, mv, u):
        S = self.S
        sk = ("lnstats", u)
        for hf in range(2):
            S.op("dve", lambda e, hf=hf: e.bn_stats(out=stats[:, hf, :], in_=tt[:, hf * 512:(hf + 1) * 512]),
                 reads=[ttkey], writes=[(sk, hf)])
        S.op("dve", lambda e: e.bn_aggr(out=mv[:, 0:2], in_=stats[:, :, :]), reads=[(sk, 0), (sk, 1)], writes=[(sk, "mv")])
        ec = self.epsc(LN_EPS)
        S.op("act", lambda e: e.activation(out=mv[:, 2:3], in_=mv[:, 1:2], func=AF.Ln, bias=ec, scale=1.0),
             reads=[(sk, "mv"), self.eps_key(LN_EPS)], writes=[(sk, "ln")])
        S.op("act", lambda e: e.activation(out=mv[:, 3:4], in_=mv[:, 2:3], func=AF.Exp, scale=-0.5),
             reads=[(sk, "ln")], writes=[(sk, "rstd")])
        S.op("dve", lambda e: e.tensor_scalar(out=xo, in0=tt, scalar1=mv[:, 0:1], scalar2=mv[:, 3:4],
                                              op0=ALU.subtract, op1=ALU.mult),
             reads=[ttkey, (sk, "mv"), (sk, "rstd")], writes=[xokey])
        S.op("dve", lambda e: e.tensor_tensor(out=xo, in0=xo, in1=lng, op=ALU.mult), reads=[xokey, lngkey], writes=[xokey])
        S.op("dve", lambda e: e.tensor_tensor(out=xo, in0=xo, in1=lnb, op=ALU.add), reads=[xokey, lnbkey], writes=[xokey])

    def epsc(self, val):
        if not hasattr(self, "_eps"):
            self._eps = {}
        if val not in self._eps:
            col = 16 + len(self._eps)
            ap = self.small[:, col:col + 1]
            self.S.op("dve", lambda e: e.memset(ap, float(val)), writes=[("epsc", col)])
            self._eps[val] = (ap, ("epsc", col))
        return self._eps[val][0]

    def eps_key(self, val):
        self.epsc(val)
        return self._eps[val][1]


A_MIX = 0
A_SCR = 36864
A_ACC = 0
A_BC = 73728
A_WORK = 98304
A_HB = 122880
A_WB = 126976


def _k_common_views(self):
    self.mix = self.view(A_MIX, (8, TT), BF16)
    self.acc = self.view(A_ACC, (NT, 1024), F32)
    self.bc = [self.view(A_BC + i * 4096, (1024,), F32) for i in range(6)]
    self.work = [self.view(A_WORK + i * 4096, (1024,), F32) for i in range(6)]
    self.hb = [self.view(A_HB + i * 2048, (1024,), BF16) for i in range(2)]


K.common_views = _k_common_views


def _tile_src(self, b, t):
    T = self.T
    if t < 2:
        return T["ctx"].ap()[b, t * 128:(t + 1) * 128, :]
    return T["x"].ap()[b, (t - 2) * 128:(t - 1) * 128, :]


K.tile_src = _tile_src


def _stats_views(self, i):
    base = 24 + i * 16
    st = self.small[:, base:base + 12].rearrange("p (a b) -> p a b", a=2)
    mv = self.small[:, base + 12:base + 16]
    return st, mv


K.stats_views = _stats_views


def _tm_tile(self, b, layer, t, mode, wr_b=None, wo=None):
    S, T = self.S, self.T
    ps = self.psum
    r = t % 2
    X, Y, W = self.work[r], self.work[2 + r], self.work[4 + r]
    kX, kY, kW = ("wX", r), ("wY", r), ("wW", r)
    hb, khb = self.hb[r], ("hb", r)
    bc = self.bc
    if mode == "s1" or (mode == "s3" and layer == 0):
        src, rk = self.tile_src(b, t), []
    else:
        src, rk = T["xres"].ap()[b, t * 128:(t + 1) * 128, :], [("xres", b, t)]
    S.dma("sp", lambda e: e.dma_start(out=X, in_=src), reads=rk, writes=[kX], semkey=kX)
    if mode == "s1":
        S.op("dve", lambda e: e.tensor_tensor(out=W, in0=X, in1=bc[0], op=ALU.mult), reads=[kX, ("bc", 0)], writes=[kW])
        S.op("dve", lambda e: e.tensor_tensor(out=hb, in0=W, in1=bc[1], op=ALU.add), reads=[kW, ("bc", 1)], writes=[khb])
    else:
        if mode == "s3":
            for n_ in range(2):
                bank = 4 + 2 * r + n_

                def mmo(e, n_=n_, bank=bank):
                    for k in range(8):
                        ins = e.matmul(ps[:, bank, :], self.mix[:, k, t * 128:(t + 1) * 128], wo[:, k, n_ * 512:(n_ + 1) * 512],
                                       start=(k == 0), stop=(k == 7))
                    return ins
                S.op("pe", mmo, reads=["wo"], writes=[("ps", bank)])
                S.op("dve", lambda e, n_=n_, bank=bank: e.tensor_tensor(
                    out=Y[:, n_ * 512:(n_ + 1) * 512], in0=ps[:, bank, :], in1=bc[0][:, n_ * 512:(n_ + 1) * 512], op=ALU.mult),
                    reads=[("ps", bank), ("bc", 0)], writes=[kY])
            ykeys = [kY]
        else:
            ti = t if layer == 0 else t - 2
            S.op("dve", lambda e: e.tensor_tensor(out=Y, in0=self.acc[:, ti, :], in1=bc[0], op=ALU.mult),
                 reads=[("bc", 0)], writes=[kY])
            ykeys = [kY]
        S.op("dve", lambda e: e.scalar_tensor_tensor(out=Y, in0=X, scalar=ALPHA, in1=Y, op0=ALU.mult, op1=ALU.add),
             reads=[kX] + ykeys, writes=[kY])
        st_, mv = self.stats_views(r)
        sk = ("lnstats", r)
        for hf in range(2):
            S.op("dve", lambda e, hf=hf: e.bn_stats(out=st_[:, hf, :], in_=Y[:, hf * 512:(hf + 1) * 512]), reads=[kY], writes=[(sk, hf)])
        S.op("dve", lambda e: e.bn_aggr(out=mv[:, 0:2], in_=st_[:, :, :]), reads=[(sk, 0), (sk, 1)], writes=[(sk, "mv")])
        ec = self.epsc(LN_EPS)
        S.op("act", lambda e: e.activation(out=mv[:, 2:3], in_=mv[:, 1:2], func=AF.Ln, bias=ec, scale=1.0),
             reads=[(sk, "mv"), self.eps_key(LN_EPS)], writes=[(sk, "ln")])
        S.op("act", lambda e: e.activation(out=mv[:, 3:4], in_=mv[:, 2:3], func=AF.Exp, scale=-0.5), reads=[(sk, "ln")], writes=[(sk, "rstd")])
        nmr = self.nmr[:, r:r + 1]
        S.op("dve", lambda e: e.scalar_tensor_tensor(out=nmr, in0=mv[:, 0:1], scalar=-1.0, in1=mv[:, 3:4], op0=ALU.mult, op1=ALU.mult),
             reads=[(sk, "mv"), (sk, "rstd")], writes=[(sk, "nmr")])
        S.op("act", lambda e: e.activation(out=Y, in_=Y, func=AF.Identity, bias=nmr, scale=mv[:, 3:4]),
             reads=[kY, (sk, "rstd"), (sk, "nmr")], writes=[kY])
        S.op("dve", lambda e: e.tensor_tensor(out=Y, in0=Y, in1=bc[1], op=ALU.mult), reads=[kY, ("bc", 1)], writes=[kY])
        S.op("dve", lambda e: e.tensor_tensor(out=Y, in0=Y, in1=bc[2], op=ALU.add), reads=[kY, ("bc", 2)], writes=[kY])
        if mode == "s5" and layer == 1:
            S.dma("sp", lambda e: e.dma_start(out=T["out"].ap()[b, (t - 2) * 128:(t - 1) * 128, :], in_=Y),
                  reads=[kY], writes=[("out", b, t)], semkey=("out_st", r))
            self.final_reads.append(("out", b, t))
            return
        if mode == "s3" or t >= 2:
            S.dma("sp", lambda e: e.dma_start(out=T["xres"].ap()[b, t * 128:(t + 1) * 128, :], in_=Y),
                  reads=[kY], writes=[("xres", b, t)], semkey=("xres_st", r))
        S.op("dve", lambda e: e.tensor_tensor(out=W, in0=Y, in1=bc[3], op=ALU.mult), reads=[kY, ("bc", 3)], writes=[kW])
        if mode == "s3" and layer == 1:
            S.op("dve", lambda e: e.tensor_tensor(out=W, in0=W, in1=bc[4], op=ALU.add), reads=[kW, ("bc", 4)], writes=[kW])
            S.op("act", lambda e: e.copy(out=hb, in_=W), reads=[kW], writes=[khb])
        else:
            S.op("dve", lambda e: e.tensor_tensor(out=hb, in0=W, in1=bc[4], op=ALU.add), reads=[kW, ("bc", 4)], writes=[khb])
    for half in range(2):
        bank = 2 * r + half

        def tr(e, half=half, bank=bank):
            for kk in range(4):
                k = half * 4 + kk
                ins = e.matmul(ps[:, bank, kk * 128:(kk + 1) * 128], hb[:, k * 128:(k + 1) * 128], self.ident[:, :], start=True, stop=True)
            return ins
        S.op("pe", tr, reads=[khb, "ident"], writes=[("ps", bank)])
        S.op("act", lambda e, half=half, bank=bank: e.copy(
            out=self.hfm[:, half * 4:(half + 1) * 4, t * 128:(t + 1) * 128], in_=ps[:, bank, :].rearrange("p (a b) -> p a b", a=4)),
            reads=[("ps", bank)], writes=[("hfm", t, half)])
    if mode == "s3" and layer == 1:
        self.router_tile(W, kW, wr_b, t - 2, X, kX, r)


K.tm_tile = _tm_tile


def _tm_phase(self, b, layer, mode, wr_b=None, wo=None):
    S = self.S
    kinds = ("ctx", "lat") if (layer == 0) else ("lat",)
    for kind in kinds:
        row = self.NB if kind == "ctx" else b
        tiles = list(range(0, 2)) if kind == "ctx" else list(range(2, NT))
        if mode == "s1":
            self.bc_load(self.bc[0], ("bc", 0), 0, row, 1)
            self.bc_load(self.bc[1], ("bc", 1), 0, row, 0)
        elif mode == "s3":
            self.bc_load(self.bc[0], ("bc", 0), layer, row, 2)
            self.bc_load(self.bc[3], ("bc", 3), layer, row, 4)
            self.bc_load(self.bc[4], ("bc", 4), layer, row, 3)
        else:
            self.bc_load(self.bc[0], ("bc", 0), layer, row, 5)
            if layer == 0:
                self.bc_load(self.bc[3], ("bc", 3), 1, row, 1)
                self.bc_load(self.bc[4], ("bc", 4), 1, row, 0)
        for i in range(0, len(tiles), 2):
            lists = []
            for t in tiles[i:i + 2]:
                S.capture()
                self.tm_tile(b, layer, t, mode, wr_b=wr_b, wo=wo)
                lists.append(S.end_capture())
            S.extend_interleaved(lists)


K.tm_phase = _tm_phase


def _s1(self, b):
    self.phase_barrier()
    self.common_views()
    self.tm_phase(b, 0, "s1")
    self.dump("hfm_l0_b%d" % b, self.hfm[:, :, :], (128, 8, TT), BF16, self.hfm_keys(0, TT))


K.s1 = _s1

TOK_TILES = [(0, 256)] + [(256 + i * 512, 512) for i in range(4)]


def _load_wst(self, dst, dkey, wap, col0, ncols=128, swap=False):
    S = self.S
    wv = wap.rearrange("(k p) n -> p k n", p=128)
    if not swap:
        S.dma("pool", lambda e: e.dma_start(out=dst[:, :, 0:ncols], in_=wv[:, :, col0:col0 + ncols]),
              writes=[dkey], semkey=dkey)
    else:
        assert ncols == 128
        for blk in range(4):
            sblk = blk ^ 1
            S.dma("pool", lambda e, blk=blk, sblk=sblk: e.dma_start(
                out=dst[:, :, blk * 32:(blk + 1) * 32],
                in_=wv[:, :, col0 + sblk * 32: col0 + (sblk + 1) * 32]),
                writes=[(dkey, blk)], semkey=(dkey, blk))


K.load_wst = _load_wst


def _proj_fm(self, wst, wkeys, t0, size, bank):
    ps = self.psum

    def mm(e):
        for k in range(8):
            ins = e.matmul(ps[:, bank, 0:size], wst[:, k, :], self.hfm[:, k, t0:t0 + size],
                           start=(k == 0), stop=(k == 7))
        return ins
    self.S.op("pe", mm, reads=list(wkeys) + self.hfm_keys(t0, size), writes=[("ps", bank)])


K.proj_fm = _proj_fm


def _rsqrt_act(self, out, in_, scale, epsval, rkeys, wkey, tmp, tmpkey):
    S = self.S
    ec = self.epsc(epsval)
    S.op("act", lambda e: e.activation(out=tmp, in_=in_, func=AF.Ln, bias=ec, scale=scale),
         reads=list(rkeys) + [self.eps_key(epsval)], writes=[tmpkey])
    S.op("act", lambda e: e.activation(out=out, in_=tmp, func=AF.Exp, scale=-0.5), reads=[tmpkey], writes=[wkey])


K.rsqrt_act = _rsqrt_act


def _s2_l0(self, b):
    S, T = self.S, self.T
    ps = self.psum
    self.common_views()
    mix = self.mix
    w_in = T["e_w_in"].ap()[0]
    wst = [self.view(A_WB + i * 2048, (8, 128), BF16) for i in range(4)]
    ubuf = self.view(A_SCR, (2308,), F32)
    bgs = self.view(A_SCR + 9232, (TT,), F32)
    ctmp = self.view(A_SCR + 9232 + 9216, (TT,), F32)
    cgs = [self.view(A_SCR + 9232 + 2 * 9216 + i * 2048, (512,), F32) for i in range(2)]
    wconv = self.view(A_WB + 20480, (3, 4), F32)
    for w_ in range(3):
        S.dma("sp", lambda e, w_=w_: e.dma_start(out=wconv[:, w_, :], in_=flat_ap(T["e_conv"], w_ * 512, [[1, 128], [128, 4]]),
                                                 allow_slow_non_contiguous=True), writes=[("wconv", w_)], semkey=("wconv", w_))
    for c0 in (0, 257, 258, 2307):
        S.op("dve", lambda e, c0=c0: e.memset(ubuf[:, c0:c0 + 1], 0.0), writes=[("upad", c0)])
    segs = [(0, 256, 1), (256, 2048, 259)]

    def ucol(t0):
        return 1 + t0 if t0 < 256 else 259 + (t0 - 256)
    for j in range(4):
        for which, colbase in (("bg", 0), ("cg", 512), ("val", 1024)):
            pass
        self.load_wst(wst[0], ("wst", 0), w_in, 0 + j * 128)
        self.load_wst(wst[1], ("wst", 1), w_in, 512 + j * 128)
        self.load_wst(wst[2], ("wst", 2), w_in, 1024 + j * 128)
        for ti, (t0, size) in enumerate(TOK_TILES):
            r = ti % 2
            self.proj_fm(wst[1], [("wst", 1)], t0, size, 0 + r)
            S.op("act", lambda e, r=r, size=size: e.copy(out=cgs[r][:, 0:size], in_=ps[:, 0 + r, 0:size]),
                 reads=[("ps", 0 + r)], writes=[("cgs", r)])
            self.proj_fm(wst[2], [("wst", 2)], t0, size, 2 + r)
            uc = ucol(t0)
            S.op("dve", lambda e, r=r, size=size, uc=uc: e.tensor_tensor(
                out=ubuf[:, uc:uc + size], in0=ps[:, 2 + r, 0:size], in1=cgs[r][:, 0:size], op=ALU.mult),
                reads=[("ps", 2 + r), ("cgs", r)], writes=[("ubuf", ti)])
            self.proj_fm(wst[0], [("wst", 0)], t0, size, 4 + r)
            S.op("act", lambda e, r=r, size=size, t0=t0: e.copy(out=bgs[:, t0:t0 + size], in_=ps[:, 4 + r, 0:size]),
                 reads=[("ps", 4 + r)], writes=[("bgs", ti)])
        for si, (t0, nt, uc) in enumerate(segs):
            ukeys = [("ubuf", ti) for ti in range(5)] + [("upad", c0) for c0 in (0, 257, 258, 2307)]
            ck = ("ctmp", si)
            S.op("dve", lambda e, t0=t0, nt=nt, uc=uc, j=j: e.tensor_scalar(
                out=ctmp[:, t0:t0 + nt], in0=ubuf[:, uc - 1:uc - 1 + nt], scalar1=wconv[:, 0, j:j + 1], scalar2=None,
                op0=ALU.mult), reads=ukeys + [("wconv", 0)], writes=[ck])
            for w_ in (1, 2):
                S.op("dve", lambda e, t0=t0, nt=nt, uc=uc, j=j, w_=w_: e.scalar_tensor_tensor(
                    out=ctmp[:, t0:t0 + nt], in0=ubuf[:, uc - 1 + w_:uc - 1 + w_ + nt], scalar=wconv[:, w_, j:j + 1],
                    in1=ctmp[:, t0:t0 + nt], op0=ALU.mult, op1=ALU.add), reads=ukeys + [("wconv", w_), ck], writes=[ck])
            S.op("dve", lambda e, t0=t0, nt=nt, j=j: e.tensor_tensor(
                out=mix[:, j, t0:t0 + nt], in0=ctmp[:, t0:t0 + nt], in1=bgs[:, t0:t0 + nt], op=ALU.mult),
                reads=[ck] + [("bgs", ti) for ti in range(5)], writes=[("mix", j, si)])
    self.dump("mixconv_b%d" % b, mix[:, 0:4, :], (128, 4, TT), BF16, [("mix", j, si) for j in range(4) for si in range(2)])
    if self.stop == "l0conv":
        return
    self.phase_barrier()
    vtm = self.view(A_SCR, (NT, 512), BF16)
    qr = self.view(A_SCR + 18432, (TT,), BF16)
    kz = [self.view(A_SCR + 18432 + 4608 + i * 4608, (TT,), BF16) for i in range(2)]
    Et = [self.view(A_SCR + 32256 + i * 1024, (512,), BF16) for i in range(4)]
    osq = self.view(A_WORK + 20480, (512,), BF16)
    esb = [self.view(A_WORK + 20480 + 1024 + i * 1024, (512,), BF16) for i in range(2)]
    wk32p = [self.view(A_HB + i * 2048, (512,), F32) for i in range(2)]
    S.op("dve", lambda e: e.memset(kz[0][64:128, :], 0.0), writes=[("kzz", 0)])
    S.op("dve", lambda e: e.memset(kz[1][0:64, :], 0.0), writes=[("kzz", 1)])
    ropec = self.view(A_BC, (L,), BF16)
    ropes = self.view(A_BC + 4096, (L,), BF16)
    wv = self.view(A_WB + 8192, (8, 512), BF16)
    wk32 = [self.view(A_WORK + i * 2048, (512,), F32) for i in range(10)]
    S.dma("sp", lambda e: e.dma_start(out=ropec, in_=T["ropec"].ap()), writes=["ropec"], semkey="ropec")
    S.dma("sp", lambda e: e.dma_start(out=ropes, in_=T["ropes"].ap()), writes=["ropes"], semkey="ropes")
    self.load_wst(wv, "wv", w_in, 2560, ncols=512)
    for t in range(NT):
        bank = t % 2

        def mmv(e, t=t, bank=bank):
            for k in range(8):
                ins = e.matmul(ps[:, bank, :], self.hfm[:, k, t * 128:(t + 1) * 128], wv[:, k, :],
                               start=(k == 0), stop=(k == 7))
            return ins
        S.op("pe", mmv, reads=["wv"] + self.hfm_keys(t * 128, 128), writes=[("ps", bank)])
        S.op("act", lambda e, t=t, bank=bank: e.copy(out=vtm[:, t, :], in_=ps[:, bank, :]),
             reads=[("ps", bank)], writes=[("vtm", t)])
    neglam = self.small[:, 8:9]
    gsc = self.small[:, 9:10]
    for hd in range(4):
        for (nm, col0, dst) in (("q", 1536 + hd * 128, qr), ("k", 2048 + hd * 128, None)):
            self.load_wst(wst[0], ("wst", 0), w_in, col0)
            self.load_wst(wst[1], ("wst", 1), w_in, col0, swap=True)
            swk = [(("wst", 1), blk) for blk in range(4)]
            for ti, (t0, size) in enumerate(TOK_TILES):
                r = ti % 2
                self.proj_fm(wst[0], [("wst", 0)], t0, size, 0 + r)
                if t0 < 256:
                    if nm == "q":
                        S.op("act", lambda e, r=r, size=size, t0=t0, dst=dst: e.copy(out=dst[:, t0:t0 + size], in_=ps[:, r, 0:size]),
                             reads=[("ps", r)], writes=[(nm, ti)])
                    else:
                        for c_ in range(2):
                            S.op("act", lambda e, r=r, size=size, t0=t0, c_=c_: e.copy(
                                out=kz[c_][c_ * 64:(c_ + 1) * 64, t0:t0 + size], in_=ps[c_ * 64:(c_ + 1) * 64, r, 0:size]),
                                reads=[("ps", r)], writes=[("k", ti, c_)])
                    continue
                self.proj_fm(wst[1], swk, t0, size, 2 + r)
                l0 = t0 - 256
                a1, a2 = wk32[2 + r], wk32[4 + r]
                S.op("dve", lambda e, r=r, l0=l0, a1=a1: e.tensor_tensor(out=a1, in0=ps[:, r, :], in1=ropec[:, l0:l0 + 512], op=ALU.mult),
                     reads=[("ps", r), "ropec"], writes=[("rp1", r)])
                S.op("dve", lambda e, r=r, l0=l0, a2=a2: e.tensor_tensor(out=a2, in0=ps[:, 2 + r, :], in1=ropes[:, l0:l0 + 512], op=ALU.mult),
                     reads=[("ps", 2 + r), "ropes"], writes=[("rz", r)])
                if nm == "q":
                    S.op("dve", lambda e, a1=a1, a2=a2, t0=t0, dst=dst: e.tensor_tensor(out=dst[:, t0:t0 + 512], in0=a1, in1=a2, op=ALU.add),
                         reads=[("rp1", r), ("rz", r)], writes=[(nm, ti)])
                else:
                    for c_ in range(2):
                        S.op("dve", lambda e, a1=a1, a2=a2, t0=t0, c_=c_: e.tensor_tensor(
                            out=kz[c_][c_ * 64:(c_ + 1) * 64, t0:t0 + 512], in0=a1[c_ * 64:(c_ + 1) * 64, :], in1=a2[c_ * 64:(c_ + 1) * 64, :], op=ALU.add),
                            reads=[("rp1", r), ("rz", r)], writes=[("k", ti, c_)])
        qkeys = lambda ti: [("q", ti)]
        allk = lambda c_: [("k", ti, c_) for ti in range(5)] + [("kzz", c_)]
        iters = []
        for ti, (t0, size) in enumerate(TOK_TILES):
            nchunks = 2 if t0 < 256 else NT
            for c in range(2):
                for j in range(nchunks):
                    iters.append((ti, t0, size, c, j, nchunks))
        sbanks = (0, 1, 3)

        def front(n, hd=hd):
            ti, t0, size, c, j, nchunks = iters[n]
            sb = sbanks[n % 3]
            pl, ph = c * 64, (c + 1) * 64
            S.op("pe", lambda e: e.matmul(ps[:, sb, 0:size], kz[c][:, j * 128:(j + 1) * 128], qr[:, t0:t0 + size], start=True, stop=True),
                 reads=allk(c) + qkeys(ti), writes=[("ps", sb)])

        def back(n, hd=hd):
            ti, t0, size, c, j, nchunks = iters[n]
            sb = sbanks[n % 3]
            es = n % 4
            bo, bz = 4 + 2 * c, 5 + 2 * c
            S.op("act", lambda e: e.activation(out=Et[es][:, 0:size], in_=ps[:, sb, 0:size], func=AF.Exp, scale=0.125),
                 reads=[("ps", sb)], writes=[("Et", es)])

            S.op("pe", lambda e: e.matmul(ps[:, bo, 0:size], vtm[:, j, hd * 128:(hd + 1) * 128], Et[es][:, 0:size],
                                          start=(j == 0), stop=(j == nchunks - 1)),
                 reads=[("Et", es), ("vtm", j)], writes=[("ps", bo)])
            par = j % 2
            eng = "dve"
            esum = wk32[2 * par + c] if par == 0 else wk32p[c]
            ek = ("esum", par, c)
            if j < 2:
                S.op(eng, lambda e: e.tensor_copy(out=esum[:, 0:size], in_=Et[es][:, 0:size]), reads=[("Et", es)], writes=[ek])
            else:
                S.op(eng, lambda e: e.tensor_tensor(out=esum[:, 0:size], in0=esum[:, 0:size], in1=Et[es][:, 0:size], op=ALU.add),
                     reads=[("Et", es), ek], writes=[ek])
            if j < nchunks - 1:
                return
            S.op("dve", lambda e: e.tensor_tensor(out=esb[c][:, 0:size], in0=wk32[c][:, 0:size], in1=wk32p[c][:, 0:size], op=ALU.add),
                 reads=[("esum", 0, c), ("esum", 1, c)], writes=[("esb", c)])
            S.op("pe", lambda e: e.matmul(ps[:, bz, 0:size], self.ones[:, :], esb[c][:, 0:size], start=True, stop=True),
                 reads=[("esb", c), "ones"], writes=[("ps", bz)])
            rz, tc_ = wk32[4 + c], wk32[6 + c]
            S.op("dve", lambda e: e.reciprocal(out=rz[:, 0:size], in_=ps[:, bz, 0:size]),
                 reads=[("ps", bz)], writes=[("rz", c)])
            S.op("dve", lambda e: e.tensor_tensor(out=tc_[:, 0:size], in0=ps[:, bo, 0:size], in1=rz[:, 0:size], op=ALU.mult),
                 reads=[("ps", bo), ("rz", c)], writes=[("tc", c)])
            if c == 0:
                return
            o_ = wk32[8]
            S.op("dve", lambda e: e.scalar_tensor_tensor(
                out=o_[:, 0:size], in0=wk32[7][:, 0:size], scalar=neglam, in1=wk32[6][:, 0:size], op0=ALU.mult, op1=ALU.add),
                reads=[("tc", 0), ("tc", 1), "neglam"], writes=["o_"])
            S.op("act", lambda e: e.activation(out=osq[:, 0:size], in_=o_[:, 0:size], func=AF.Square),
                 reads=["o_"], writes=["osq"])
            S.op("pe", lambda e: e.matmul(ps[:, 2, 0:size], self.ones[:, :], osq[:, 0:size], start=True, stop=True),
                 reads=["osq", "ones"], writes=[("ps", 2)])
            rs_, ltmp = wk32[9], wk32[5]
            self.rsqrt_act(rs_[:, 0:size], ps[:, 2, 0:size], 1.0 / 128.0, RMS_EPS, [("ps", 2)], "rs_", ltmp[:, 0:size], ("rz", 1))
            S.op("dve", lambda e: e.scalar_tensor_tensor(
                out=mix[:, 4 + hd, t0:t0 + size], in0=o_[:, 0:size], scalar=gsc, in1=rs_[:, 0:size], op0=ALU.mult, op1=ALU.mult),
                reads=["o_", "rs_", "gsc"], writes=[("mixa", hd, ti)])
        NI = len(iters)
        DPIPE = 2
        for n in range(min(DPIPE, NI)):
            front(n)
        for n in range(NI):
            if n + DPIPE < NI:
                front(n + DPIPE)
            back(n)
    self.dump("mixattn_b%d" % b, mix[:, 4:8, :], (128, 4, TT), BF16, [("mixa", hd, ti) for hd in range(4) for ti in range(5)])


K.s2_l0 = _s2_l0


def _s3(self, b, layer):
    S, T = self.S, self.T
    ps = self.psum
    self.phase_barrier()
    self.common_views()
    mix = self.mix
    wo = self.view(A_WB, (8, 1024), BF16)
    wname = "e_w_o" if layer == 0 else "o_w_o"
    S.dma("pool", lambda e: e.dma_start(out=wo[:, :, :], in_=T[wname].ap()[0].rearrange("(k p) n -> p k n", p=128)),
          writes=["wo"], semkey="wo")
    self.bc_load_ln(self.bc[1], ("bc", 1), "ln_g", layer, 0)
    self.bc_load_ln(self.bc[2], ("bc", 2), "ln_b", layer, 0)
    kinds = ("ctx", "lat") if layer == 0 else ("lat",)
    if layer == 1:
        wr32 = self.view(A_SCR, (8, 8), F32)
        wr32b = self.view(A_SCR + 256, (8, 8), F32)
        whi = self.view(A_SCR + 512, (8, 8), BF16)
        wlo = self.view(A_SCR + 640, (8, 8), BF16)
        lob = [self.view(A_SCR + 1024 + i * 2048, (1024,), BF16) for i in range(2)]
        lofm = [self.view(A_SCR + 1024 + 4096 + i * 2048, (8, 128), BF16) for i in range(2)]
        S.dma("sp", lambda e: e.dma_start(out=wr32[:, :, :], in_=T["o_router"].ap()[0].rearrange("(k p) e -> p k e", p=128)),
              writes=["wr32"], semkey="wr32")
        S.op("dve", lambda e: e.tensor_copy(out=whi[:, :, :], in_=wr32[:, :, :]), reads=["wr32"], writes=["whi"])
        S.op("dve", lambda e: e.tensor_tensor(out=wr32b[:, :, :], in0=wr32[:, :, :], in1=whi[:, :, :], op=ALU.subtract),
             reads=["wr32", "whi"], writes=["wr32b"])
        S.op("dve", lambda e: e.tensor_copy(out=wlo[:, :, :], in_=wr32b[:, :, :]), reads=["wr32b"], writes=["wlo"])
        wr_b = (whi, wlo, lob, lofm)
    self.tm_phase(b, layer, "s3", wr_b=(wr_b if layer == 1 else None), wo=wo)
    self.dump("xres_l%d_b%d" % (layer, b), T["xres"].ap()[b], (TT, D), F32, [("xres", b, t) for t in range(NT)])
    self.dump("hfm2_l%d_b%d" % (layer, b), self.hfm[:, :, :], (128, 8, TT), BF16, self.hfm_keys(0, TT))
    if layer == 1:
        self.dump("gates_b%d" % b, self.gates[:, :, :], (128, 16, 8), F32, [("gates", lt) for lt in range(16)])


K.s3 = _s3


def _router_tile(self, h32, hkey, wr_b, lt, junk, junkkey, r):
    S = self.S
    ps = self.psum
    whi, wlo, lob_, lofm_ = wr_b
    lob, lofm = lob_[r], lofm_[r]
    hb, khb = self.hb[r], ("hb", r)
    t = lt + 2
    lg = self.rts[:, r, 0:8]
    u = ("rt", r)
    S.op("dve", lambda e: e.tensor_tensor(out=lob, in0=h32, in1=hb, op=ALU.subtract), reads=[hkey, khb], writes=[("lob", r)])
    for half in range(2):
        bank = 4 + 2 * r + half

        def trl(e, half=half, bank=bank):
            for kk in range(4):
                k = half * 4 + kk
                ins = e.matmul(ps[:, bank, kk * 128:(kk + 1) * 128], lob[:, k * 128:(k + 1) * 128], self.ident[:, :], start=True, stop=True)
            return ins
        S.op("pe", trl, reads=[("lob", r), "ident"], writes=[("ps", bank)])
        S.op("act", lambda e, half=half, bank=bank: e.copy(out=lofm[:, half * 4:(half + 1) * 4, :],
                                                          in_=ps[:, bank, :].rearrange("p (a b) -> p a b", a=4)),
             reads=[("ps", bank)], writes=[("lofm", r, half)])
    lbank = 4 + 2 * r

    def mml(e):
        n_ = 0
        for k in range(8):
            for (a_, w_) in ((self.hfm[:, k, t * 128:(t + 1) * 128], whi), (self.hfm[:, k, t * 128:(t + 1) * 128], wlo), (lofm[:, k, :], whi)):
                ins = e.matmul(ps[:, lbank, 0:8], a_, w_[:, k, :], start=(n_ == 0), stop=(n_ == 23))
                n_ += 1
        return ins
    S.op("pe", mml, reads=[("hfm", t, 0), ("hfm", t, 1), ("lofm", r, 0), ("lofm", r, 1), "whi", "wlo"], writes=[("ps", lbank)])
    S.op("dve", lambda e: e.tensor_copy(out=lg, in_=ps[:, lbank, 0:8]), reads=[("ps", lbank)], writes=[(u, "lg", e_) for e_ in range(8)])
    lgk = [(u, "lg", e_) for e_ in range(8)]
    g = self.gates[:, lt, :]
    w8 = self.rts[:, r, 8:32].rearrange("p (a b) -> p a b", a=3)
    m1, m2, dd, ee, p1, p2 = (self.rts[:, r, 32 + i:33 + i] for i in range(6))
    S.op("dve", lambda e: e.reduce_max(out=m1, in_=lg, axis=AX.X), reads=lgk, writes=[(u, "m1")])
    S.op("dve", lambda e: e.tensor_scalar(out=w8[:, 0, :], in0=lg, scalar1=m1, scalar2=None, op0=ALU.is_equal),
         reads=lgk + [(u, "m1")], writes=[(u, "eq1")])
    S.op("dve", lambda e: e.scalar_tensor_tensor(out=w8[:, 1, :], in0=w8[:, 0, :], scalar=-1e30, in1=lg, op0=ALU.mult, op1=ALU.add),
         reads=lgk + [(u, "eq1")], writes=[(u, "l2")])
    S.op("dve", lambda e: e.reduce_max(out=m2, in_=w8[:, 1, :], axis=AX.X), reads=[(u, "l2")], writes=[(u, "m2")])
    S.op("dve", lambda e: e.tensor_scalar(out=w8[:, 2, :], in0=w8[:, 1, :], scalar1=m2, scalar2=None, op0=ALU.is_equal),
         reads=[(u, "l2"), (u, "m2")], writes=[(u, "eq2")])
    S.op("dve", lambda e: e.tensor_tensor(out=dd, in0=m2, in1=m1, op=ALU.subtract), reads=[(u, "m1"), (u, "m2")], writes=[(u, "dd")])
    S.op("act", lambda e: e.activation(out=ee, in_=dd, func=AF.Exp), reads=[(u, "dd")], writes=[(u, "ee")])
    S.op("dve", lambda e: e.tensor_scalar(out=p2, in0=ee, scalar1=1.0, scalar2=None, op0=ALU.add), reads=[(u, "ee")], writes=[(u, "den")])
    S.op("dve", lambda e: e.reciprocal(out=p1, in_=p2), reads=[(u, "den")], writes=[(u, "p1")])
    S.op("dve", lambda e: e.tensor_tensor(out=p2, in0=ee, in1=p1, op=ALU.mult), reads=[(u, "ee"), (u, "p1"), (u, "den")], writes=[(u, "p2")])
    S.op("dve", lambda e: e.tensor_scalar(out=w8[:, 0, :], in0=w8[:, 0, :], scalar1=p1, scalar2=None, op0=ALU.mult),
         reads=[(u, "eq1"), (u, "p1")], writes=[(u, "g1")])
    S.op("dve", lambda e: e.scalar_tensor_tensor(out=g, in0=w8[:, 2, :], scalar=p2, in1=w8[:, 0, :], op0=ALU.mult, op1=ALU.add),
         reads=[(u, "eq2"), (u, "p2"), (u, "g1")], writes=[("gates", lt)])


K.router_tile = _router_tile


def _swiglu(self, b, experts, tiles, first_tile):
    S = self.S
    ps = self.psum
    self.phase_barrier()
    self.common_views()
    acc = self.acc
    base = A_BC
    wsets = []
    for i in range(2):
        o = base + i * 24576
        wsets.append((self.view(o, (8, 512), BF16), self.view(o + 8192, (8, 512), BF16), self.view(o + 16384, (4, 1024), BF16)))
    hid = [self.view(base + 49152 + i * 4096, (4, 512), BF16) for i in range(2)]
    sg = [self.view(base + 49152 + 8192 + i * 1024, (512,), BF16) for i in range(2)]
    gi = 0
    first = True
    stages = []
    for (wg_ap, wu_ap, wd_ap, F, gcol) in experts:
        nch = F // 128
        c0 = 0
        while c0 < nch:
            ng = min(4, nch - c0)
            ws = gi % 2
            wg, wu, wd = wsets[ws]
            wgk, wuk, wdk = ("wg", ws), ("wu", ws), ("wd", ws)
            cols = slice(c0 * 128, (c0 + ng) * 128)
            S.capture()
            S.dma("pool", lambda e, wg=wg, wg_ap=wg_ap, cols=cols, ng=ng: e.dma_start(
                out=wg[:, :, 0:ng * 128], in_=wg_ap.rearrange("(k p) n -> p k n", p=128)[:, :, cols]),
                writes=[wgk], semkey=wgk)
            S.dma("pool", lambda e, wu=wu, wu_ap=wu_ap, cols=cols, ng=ng: e.dma_start(
                out=wu[:, :, 0:ng * 128], in_=wu_ap.rearrange("(k p) n -> p k n", p=128)[:, :, cols]),
                writes=[wuk], semkey=wuk)
            S.dma("pool", lambda e, wd=wd, wd_ap=wd_ap, c0=c0, ng=ng: e.dma_start(
                out=wd[:, 0:ng, :], in_=wd_ap[c0 * 128:(c0 + ng) * 128, :].rearrange("(c p) n -> p c n", p=128)),
                writes=[wdk], semkey=wdk)
            wlist = S.end_capture()
            for ti, (t0, size) in enumerate(tiles):
                hs = len(stages) % 2
                hd_, hk = hid[hs], ("hid", hs)
                S.capture()
                for c in range(ng):
                    pr = c % 2
                    bg_, bu_ = 0 + pr, 2 + pr

                    def mmg(e, c=c, bg_=bg_, bu_=bu_, t0=t0, size=size, wg=wg, wu=wu):
                        for k in range(8):
                            e.matmul(ps[:, bg_, 0:size], wg[:, k, c * 128:(c + 1) * 128], self.hfm[:, k, t0:t0 + size],
                                     start=(k == 0), stop=(k == 7))
                        for k in range(8):
                            ins = e.matmul(ps[:, bu_, 0:size], wu[:, k, c * 128:(c + 1) * 128], self.hfm[:, k, t0:t0 + size],
                                           start=(k == 0), stop=(k == 7))
                        return ins
                    S.op("pe", mmg, reads=[wgk, wuk] + self.hfm_keys(t0, size), writes=[("ps", bg_), ("ps", bu_)])
                    S.op("act", lambda e, pr=pr, bg_=bg_, size=size: e.activation(out=sg[pr][:, 0:size], in_=ps[:, bg_, 0:size], func=AF.Silu),
                         reads=[("ps", bg_)], writes=[("sg", pr)])
                    S.op("dve", lambda e, pr=pr, bu_=bu_, size=size, c=c, hd_=hd_: e.tensor_tensor(
                        out=hd_[:, c, 0:size], in0=ps[:, bu_, 0:size], in1=sg[pr][:, 0:size], op=ALU.mult),
                        reads=[("ps", bu_), ("sg", pr)], writes=[(hk, c)])
                glist = S.end_capture()
                S.capture()
                for s_ in range(size // 128):
                    tile_i = (t0 // 128) + s_ - first_tile
                    for n_ in range(2):
                        bank = 4 + (2 * s_ + n_) % 4

                        def mmd(e, s_=s_, n_=n_, bank=bank, hd_=hd_, wd=wd, ng=ng):
                            for c in range(ng):
                                ins = e.matmul(ps[:, bank, :], hd_[:, c, s_ * 128:(s_ + 1) * 128], wd[:, c, n_ * 512:(n_ + 1) * 512],
                                               start=(c == 0), stop=(c == ng - 1))
                            return ins
                        S.op("pe", mmd, reads=[wdk] + [(hk, c) for c in range(ng)], writes=[("ps", bank)])
                        a_ = acc[:, tile_i, n_ * 512:(n_ + 1) * 512]
                        ak = ("acc", tile_i, n_)
                        if gcol is None:
                            if first:
                                S.op("dve", lambda e, a_=a_, bank=bank: e.tensor_copy(out=a_, in_=ps[:, bank, :]),
                                     reads=[("ps", bank)], writes=[ak])
                            else:
                                S.op("dve", lambda e, a_=a_, bank=bank: e.tensor_tensor(out=a_, in0=ps[:, bank, :], in1=a_, op=ALU.add),
                                     reads=[("ps", bank), ak], writes=[ak])
                        else:
                            gsc_ = self.gates[:, tile_i, gcol:gcol + 1]
                            if first:
                                S.op("dve", lambda e, a_=a_, bank=bank, gsc_=gsc_: e.tensor_scalar(
                                    out=a_, in0=ps[:, bank, :], scalar1=gsc_, scalar2=None, op0=ALU.mult),
                                    reads=[("ps", bank), ("gates", tile_i)], writes=[ak])
                            else:
                                S.op("dve", lambda e, a_=a_, bank=bank, gsc_=gsc_: e.scalar_tensor_tensor(
                                    out=a_, in0=ps[:, bank, :], scalar=gsc_, in1=a_, op0=ALU.mult, op1=ALU.add),
                                    reads=[("ps", bank), ak, ("gates", tile_i)], writes=[ak])
                dlist = S.end_capture()
                stages.append((wlist if ti == 0 else None, glist, dlist))
            first = False
            gi += 1
            c0 += ng
    NS = len(stages)
    for n in range(NS):
        if n == 0:
            S.ops.extend(stages[0][0])
            S.ops.extend(stages[0][1])
        if n + 1 < NS:
            if stages[n + 1][0] is not None:
                S.ops.extend(stages[n + 1][0])
            S.ops.extend(stages[n + 1][1])
        S.ops.extend(stages[n][2])


K.swiglu = _swiglu


def _s4_l0(self, b):
    T = self.T
    self.swiglu(b, [(T["e_ffn_gate"].ap()[0], T["e_ffn_up"].ap()[0], T["e_ffn_down"].ap()[0], D_FF, None)],
                TOK_TILES, 0)


K.s4_l0 = _s4_l0


def _s4_l1(self, b):
    T = self.T
    ex = [(T["o_exp_gate"].ap()[0, e_], T["o_exp_up"].ap()[0, e_], T["o_exp_down"].ap()[0, e_], D_FFE, e_) for e_ in range(N_EXP)]
    self.swiglu(b, ex, TOK_TILES[1:], 2)


K.s4_l1 = _s4_l1


def _s5(self, b, layer):
    S, T = self.S, self.T
    self.phase_barrier()
    self.common_views()
    acc = self.acc
    self.bc_load_ln(self.bc[1], ("bc", 1), "ln_g", layer, 1)
    self.bc_load_ln(self.bc[2], ("bc", 2), "ln_b", layer, 1)
    self.tm_phase(b, layer, "s5")
    if layer == 0:
        self.dump("xres5_b%d" % b, T["xres"].ap()[b], (TT, D), F32, [("xres", b, t) for t in range(2, NT)])
        self.dump("hfm5_b%d" % b, self.hfm[:, :, :], (128, 8, TT), BF16, self.hfm_keys(0, TT))


K.s5 = _s5


def _layer0(self, b):
    self.s1(b)
    if self.stop == "l0s1":
        return
    self.s2_l0(b)
    if self.stop in ("l0conv", "l0s2"):
        return
    self.s3(b, 0)
    if self.stop == "l0s3":
        return
    self.s4_l0(b)
    self.s5(b, 0)


K.layer0 = _layer0


LAT_TILES = TOK_TILES[1:]


def _s2_l1(self, b):
    S, T = self.S, self.T
    ps = self.psum
    self.phase_barrier()
    self.common_views()
    mix = self.mix
    w_in = T["o_w_in"].ap()[0]
    B0 = A_SCR
    gfm = self.view(B0, (4, L), BF16)
    Atm = self.view(B0 + 16384, (16, 512), BF16)
    Btm = self.view(B0 + 32768, (16, 512), BF16)
    tabs = [(self.view(B0 + 49152 + i * 8192, (16, 128), BF16), self.view(B0 + 49152 + i * 8192 + 4096, (16, 128), BF16)) for i in range(2)]
    o2 = B0 + 49152 + 16384
    pcb = [self.view(o2 + i * 1024, (512,), BF16) for i in range(2)]
    sqb = [self.view(o2 + 2048 + i * 1024, (512,), BF16) for i in range(2)]
    ftm = [self.view(o2 + 4096 + i * 1024, (512,), BF16) for i in range(2)]
    f32t = [self.view(o2 + 6144 + i * 2048, (512,), F32) for i in range(8)]
    wst = [self.view(o2 + 6144 + 16384 + i * 2048, (8, 128), BF16) for i in range(4)]
    ccs = self.view(o2 + 6144 + 16384 + 8192, (2, 128), BF16)
    S.dma("sp", lambda e: e.dma_start(out=ccs[:, 0, :], in_=T["dft_cc"].ap()), writes=[("ccs", 0)], semkey=("ccs", 0))
    S.dma("sp", lambda e: e.dma_start(out=ccs[:, 1, :], in_=T["dft_sc"].ap()), writes=[("ccs", 1)], semkey=("ccs", 1))
    for g in range(4):
        self.load_wst(wst[g % 2], ("wst", g % 2), w_in, g * 128)
        for ti, (t0, size) in enumerate(LAT_TILES):
            r = ti % 2
            l0 = t0 - 256
            self.proj_fm(wst[g % 2], [("wst", g % 2)], t0, size, r)
            S.op("act", lambda e, r=r: e.copy(out=pcb[r], in_=ps[:, r, :]), reads=[("ps", r)], writes=[("pcb", r)])
            S.op("act", lambda e, r=r: e.activation(out=sqb[r], in_=ps[:, r, :], func=AF.Square), reads=[("ps", r)], writes=[("sqb", r)])
            S.op("pe", lambda e, r=r: e.matmul(ps[:, 2 + r, :], self.ones[:, :], pcb[r], start=True, stop=True),
                 reads=[("pcb", r), "ones"], writes=[("ps", 2 + r)])
            S.op("pe", lambda e, r=r: e.matmul(ps[:, 4 + r, :], self.ones[:, :], sqb[r], start=True, stop=True),
                 reads=[("sqb", r), "ones"], writes=[("ps", 4 + r)])
            mean, m2, var, ltmp, rstd, cen = (f32t[i] for i in (0 + r, 2 + r, 2 + r, 4 + r, 4 + r, 6 + r))
            S.op("dve", lambda e, r=r, mean=mean: e.tensor_scalar(out=mean, in0=ps[:, 2 + r, :], scalar1=1.0 / 128, scalar2=None, op0=ALU.mult),
                 reads=[("ps", 2 + r)], writes=[("f32t", 0 + r)])
            S.op("dve", lambda e, mean=mean, m2=m2: e.tensor_tensor(out=m2, in0=mean, in1=mean, op=ALU.mult),
                 reads=[("f32t", 0 + r)], writes=[("f32t", 2 + r)])
            S.op("dve", lambda e, r=r, m2=m2, var=var: e.scalar_tensor_tensor(out=var, in0=ps[:, 4 + r, :], scalar=1.0 / 128, in1=m2, op0=ALU.mult, op1=ALU.subtract),
                 reads=[("ps", 4 + r), ("f32t", 2 + r)], writes=[("f32t", 2 + r)])
            self.rsqrt_act(rstd, var, 1.0, LN_EPS, [("f32t", 2 + r)], ("f32t", 4 + r), ltmp, ("f32t", 4 + r))
            S.op("dve", lambda e, r=r, cen=cen, mean=mean: e.tensor_tensor(out=cen, in0=ps[:, r, :], in1=mean, op=ALU.subtract),
                 reads=[("ps", r), ("f32t", 0 + r)], writes=[("f32t", 6 + r)])
            S.op("dve", lambda e, cen=cen, rstd=rstd, g=g, l0=l0: e.tensor_tensor(out=gfm[:, g, l0:l0 + 512], in0=cen, in1=rstd, op=ALU.mult),
                 reads=[("f32t", 6 + r), ("f32t", 4 + r)], writes=[("gfm", g, ti)])
    gk = [("gfm", g, ti) for g in range(4) for ti in range(4)]
    self.dump("gfm_b%d" % b, gfm[:, :, :], (128, 4, L), BF16, gk)
    if self.stop == "l1fa":
        return
    for t in range(16):
        r = t % 2

        def mmab(e, t=t, r=r):
            for g in range(4):
                e.matmul(ps[:, r, g * 128:(g + 1) * 128], gfm[:, g, t * 128:(t + 1) * 128], ccs[:, 0, :], start=True, stop=True)
            for g in range(4):
                ins = e.matmul(ps[:, 2 + r, g * 128:(g + 1) * 128], gfm[:, g, t * 128:(t + 1) * 128], ccs[:, 1, :], start=True, stop=True)
            return ins
        S.op("pe", mmab, reads=gk + [("ccs", 0), ("ccs", 1)], writes=[("ps", r), ("ps", 2 + r)])
        S.op("act", lambda e, t=t, r=r: e.copy(out=Atm[:, t, :], in_=ps[:, r, :]), reads=[("ps", r)], writes=[("Atm", t)])
        S.op("dve", lambda e, t=t, r=r: e.tensor_copy(out=Btm[:, t, :], in_=ps[:, 2 + r, :]), reads=[("ps", 2 + r)], writes=[("Btm", t)])
    abk = [("Atm", t) for t in range(16)] + [("Btm", t) for t in range(16)]
    self.dump("atm_b%d" % b, Atm[:, :, :], (128, 16, 512), BF16, abk)
    if self.stop == "l1fb":
        return
    for mt in range(16):
        r = mt % 2
        cl, sl = tabs[r]
        S.dma("sp", lambda e, cl=cl, mt=mt: e.dma_start(out=cl[:, :, :], in_=T["dft_cl"].ap().rearrange("(c p) m -> p c m", p=128)[:, :, mt * 128:(mt + 1) * 128]),
              writes=[("cl", r)], semkey=("cl", r))
        S.dma("sp", lambda e, sl=sl, mt=mt: e.dma_start(out=sl[:, :, :], in_=T["dft_sl"].ap().rearrange("(c p) m -> p c m", p=128)[:, :, mt * 128:(mt + 1) * 128]),
              writes=[("sl", r)], semkey=("sl", r))

        def mmf(e, cl=cl, sl=sl, r=r):
            for c_ in range(16):
                e.matmul(ps[:, 4 + r, :], cl[:, c_, :], Atm[:, c_, :], start=(c_ == 0), stop=False)
            for c_ in range(16):
                ins = e.matmul(ps[:, 4 + r, :], sl[:, c_, :], Btm[:, c_, :], start=False, stop=(c_ == 15))
            return ins
        S.op("pe", mmf, reads=abk + [("cl", r), ("sl", r)], writes=[("ps", 4 + r)])
        S.op("act", lambda e, r=r: e.mul(out=ftm[r], in_=ps[:, 4 + r, :], mul=1.0 / 512),
             reads=[("ps", 4 + r)], writes=[("ftm", r)])

        def trf(e, r=r):
            for g in range(4):
                ins = e.matmul(ps[:, 6 + r, g * 128:(g + 1) * 128], ftm[r][:, g * 128:(g + 1) * 128], self.ident[:, :], start=True, stop=True)
            return ins
        S.op("pe", trf, reads=[("ftm", r), "ident"], writes=[("ps", 6 + r)])
        S.op("act", lambda e, r=r, mt=mt: e.copy(out=mix[:, 0:4, 256 + mt * 128: 256 + (mt + 1) * 128],
                                                in_=ps[:, 6 + r, :].rearrange("p (a b) -> p a b", a=4)),
             reads=[("ps", 6 + r)], writes=[("mixf", mt)])
    self.dump("mixfour_b%d" % b, mix[:, 0:4, :], (128, 4, TT), BF16, [("mixf", mt) for mt in range(16)])
    if self.stop == "l1four":
        return
    self.phase_barrier()
    qz = [self.view(B0, (4, L), BF16), self.view(B0 + 16384 + 2 * 18432 + 8192 + 15360 + 12288 + 4096 + 8192, (4, L), BF16)]
    S.op("dve", lambda e: e.memset(qz[0][64:128, :, :], 0.0), writes=[("qzz", 0)])
    S.op("dve", lambda e: e.memset(qz[1][0:64, :, :], 0.0), writes=[("qzz", 1)])
    kfm = self.view(B0 + 16384, (4, TT), BF16)
    vtm = self.view(B0 + 16384 + 18432, (NT, 512), BF16)
    o3 = B0 + 16384 + 2 * 18432
    EB = self.view(o3, (2, 8, 256), BF16)
    vtm2 = self.view(o3 + 8192, (15, 512), BF16)
    o4 = o3 + 8192 + 15360
    rev = [self.view(o4 + i * 1024, (256,), F32) for i in range(2)]
    ebt = [self.view(o4 + 2048 + i * 1024, (256,), F32) for i in range(2)]
    nav = self.view(o4 + 4096, (4, 64), F32)
    jrev = self.view(o4 + 5120, (128,), F32)
    zt = self.view(o4 + 5632, (640,), F32)
    Eb = [self.view(o4 + 8192 + i * 768, (384,), BF16) for i in range(4)]
    rzt = [self.view(o4 + 8192 + 3072 + i * 256, (64,), F32) for i in range(2)]
    wst = [self.view(o4 + 12288 + i * 2048, (8, 128), BF16) for i in range(2)]
    wv = self.view(o4 + 12288 + 4096, (8, 512), BF16)
    assert o4 + 12288 + 4096 + 8192 + 16384 <= self.ARENA_BYTES, o4
    S.op("dve", lambda e: e.memset(zt[0:8, :], 0.0), writes=["zt"])
    S.dma("sp", lambda e: e.dma_start(out=T["rpbp"].ap(), in_=zt[0:8, :]), reads=["zt"], writes=["rpbp"], semkey="rpbp0")
    S.dma("sp", lambda e: e.dma_start(out=T["rpbp"].ap()[:, 64:64 + 465], in_=T["rpbf"].ap()), reads=["rpbp"], writes=["rpbp"], semkey="rpbp1")
    for cb in range(4):
        S.dma("sp", lambda e, cb=cb: e.dma_start(out=nav[:, cb, :], in_=T["navalid"].ap()), writes=[("nav", cb)], semkey=("nav", cb))
    S.dma("sp", lambda e: e.dma_start(out=jrev, in_=T["jrev"].ap()), writes=["jrev"], semkey="jrev")
    navk = [("nav", cb) for cb in range(4)]
    def build_eb(h):
        for pi in range(8):
            r = (h * 8 + pi) % 2
            for hi in range(2):
                off = h * 640 + 64 + (pi + hi) * 31 - 48
                S.dma("sp", lambda e, r=r, hi=hi, off=off: e.dma_start(
                    out=rev[r][hi * 64:(hi + 1) * 64, :].rearrange("p (a b) -> p a b", a=4),
                    in_=flat_ap(T["rpbp"], off, [[1, 64], [62, 4], [1, 64]])),
                    reads=["rpbp"], writes=[("rev", r, hi)], semkey=("rev", r, hi))
            S.op("pe", lambda e, r=r: e.matmul(ps[:, 6 + r, 0:256], jrev, rev[r], start=True, stop=True),
                 reads=[("rev", r, 0), ("rev", r, 1), "jrev"], writes=[("ps", 6 + r)])
            S.op("act", lambda e, r=r: e.activation(out=ebt[r], in_=ps[:, 6 + r, 0:256], func=AF.Exp), reads=[("ps", 6 + r)], writes=[("ebt", r)])
            S.op("dve", lambda e, r=r, h=h, pi=pi: e.tensor_tensor(out=EB[:, h % 2, pi, :], in0=ebt[r], in1=nav[:, :, :].rearrange("p a b -> p (a b)"), op=ALU.mult),
                 reads=[("ebt", r)] + navk, writes=[("EB", h % 2, pi)])
    for j in range(4):
        self.load_wst(wst[j % 2], ("wst", j % 2), w_in, 512 + j * 128)
        for ti, (t0, size) in enumerate(LAT_TILES):
            r = ti % 2
            self.proj_fm(wst[j % 2], [("wst", j % 2)], t0, size, 2 + r)
            for c_ in range(2):
                S.op("act", lambda e, r=r, j=j, t0=t0, c_=c_: e.copy(out=qz[c_][c_ * 64:(c_ + 1) * 64, j, t0 - 256:t0 - 256 + 512],
                                                                  in_=ps[c_ * 64:(c_ + 1) * 64, 2 + r, :]),
                     reads=[("ps", 2 + r)], writes=[("qfm", j, ti, c_)])
    for j in range(4):
        self.load_wst(wst[j % 2], ("wst", j % 2), w_in, 1024 + j * 128)
        for ti, (t0, size) in enumerate(TOK_TILES):
            r = ti % 2
            self.proj_fm(wst[j % 2], [("wst", j % 2)], t0, size, 2 + r)
            S.op("act", lambda e, r=r, j=j, t0=t0, size=size: e.copy(out=kfm[:, j, t0:t0 + size], in_=ps[:, 2 + r, 0:size]),
                 reads=[("ps", 2 + r)], writes=[("kfm", j, ti)])
    self.load_wst(wv, "wv", w_in, 1536, ncols=512)
    for t in range(NT):
        bank = 4 + t % 2

        def mmv(e, t=t, bank=bank):
            for k in range(8):
                ins = e.matmul(ps[:, bank, :], self.hfm[:, k, t * 128:(t + 1) * 128], wv[:, k, :], start=(k == 0), stop=(k == 7))
            return ins
        S.op("pe", mmv, reads=["wv"] + self.hfm_keys(t * 128, 128), writes=[("ps", bank)])
        S.op("act", lambda e, t=t, bank=bank: e.copy(out=vtm[:, t, :], in_=ps[:, bank, :]), reads=[("ps", bank)], writes=[("vtm", t)])
    for t in range(15):
        bank = 4 + t % 2

        def mmv2(e, t=t, bank=bank):
            for k in range(8):
                ins = e.matmul(ps[:, bank, :], self.hfm[:, k, 320 + t * 128:320 + (t + 1) * 128], wv[:, k, :], start=(k == 0), stop=(k == 7))
            return ins
        S.op("pe", mmv2, reads=["wv"] + self.hfm_keys(320 + t * 128, 128), writes=[("ps", bank)])
        S.op("act", lambda e, t=t, bank=bank: e.copy(out=vtm2[:, t, :], in_=ps[:, bank, :]), reads=[("ps", bank)], writes=[("vtm2", t)])
    rows = [(j, c, r_) for j in range(4) for c in range(2) for r_ in range(32)]
    sbanks = (0, 1, 4)

    def kblocks(r_):
        rs = min(max(r_ - 4, 0), 24)
        kb = []
        for cb in range(4):
            row0 = rs + 2 * (3 - cb)
            if row0 % 2 == 0:
                kb.append((256 + row0 * 64, vtm[:, 2 + row0 // 2, :], ("vtm", 2 + row0 // 2)))
            else:
                kb.append((256 + row0 * 64, vtm2[:, (row0 - 1) // 2, :], ("vtm2", (row0 - 1) // 2)))
        kb += [(0, vtm[:, 0, :], ("vtm", 0)), (128, vtm[:, 1, :], ("vtm", 1))]
        return kb, r_ - rs

    def nfront(n):
        j, c, r_ = rows[n]
        if r_ == 0:
            build_eb(2 * j + c)
        sb = sbanks[n % 3]
        pl, ph = c * 64, (c + 1) * 64
        kb, _ = kblocks(r_)

        def mms(e):
            for blk, (kc0, _, _) in enumerate(kb):
                ins = e.matmul(ps[:, sb, blk * 64:(blk + 1) * 64], kfm[:, j, kc0:kc0 + 128],
                               qz[c][:, j, r_ * 64:(r_ + 1) * 64], start=True, stop=True)
            return ins
        S.op("pe", mms, reads=[("qfm", j, ti, c) for ti in range(4)] + [("qzz", c)] + [("kfm", j, ti) for ti in range(5)], writes=[("ps", sb)])

    def stageB(n):
        j, c, r_ = rows[n]
        h = 2 * j + c
        sb = sbanks[n % 3]
        es = n % 4
        kb, pi = kblocks(r_)
        S.op("act", lambda e: e.activation(out=Eb[es], in_=ps[:, sb, 0:384], func=AF.Exp, scale=0.125),
             reads=[("ps", sb)], writes=[("Eb", es)])
        S.op("dve", lambda e: e.tensor_tensor(out=Eb[es][:, 0:256], in0=Eb[es][:, 0:256], in1=EB[:, h % 2, pi, :], op=ALU.mult),
             reads=[("Eb", es), ("EB", h % 2, pi)], writes=[("Eb", es)])

    def stageC(n):
        j, c, r_ = rows[n]
        ob = (2, 3, 5)[n % 3]
        es = n % 4
        kb, pi = kblocks(r_)

        def mmo(e):
            for blk, (_, vt, _) in enumerate(kb):
                e.matmul(ps[:, ob, 0:64], vt[:, j * 128:(j + 1) * 128], Eb[es][:, blk * 64:(blk + 1) * 64],
                         start=(blk == 0), stop=(blk == 5))
            for blk in range(6):
                ins = e.matmul(ps[:, ob, 64:128], self.ones[:, :], Eb[es][:, blk * 64:(blk + 1) * 64],
                               start=(blk == 0), stop=(blk == 5))
            return ins
        S.op("pe", mmo, reads=[("Eb", es), "ones"] + [vk for (_, _, vk) in kb], writes=[("ps", ob)])

    def stageD(n):
        j, c, r_ = rows[n]
        h = 2 * j + c
        ob = (2, 3, 5)[n % 3]
        pl, ph = c * 64, (c + 1) * 64
        rz = rzt[n % 2]
        S.op("dve", lambda e: e.reciprocal(out=rz[pl:ph, :], in_=ps[pl:ph, ob, 64:128]),
             reads=[("ps", ob)], writes=[("rzt", n % 2)])
        S.op("dve", lambda e: e.tensor_tensor(
            out=mix[pl:ph, 4 + j, 256 + r_ * 64:256 + (r_ + 1) * 64], in0=ps[pl:ph, ob, 0:64], in1=rz[pl:ph, :], op=ALU.mult),
            reads=[("ps", ob), ("rzt", n % 2)], writes=[("mixn", h, r_)])
    NR = len(rows)
    for step in range(NR + 3):
        if step < NR:
            nfront(step)
        if 0 <= step - 1 < NR:
            stageB(step - 1)
        if 0 <= step - 2 < NR:
            stageC(step - 2)
        if 0 <= step - 3 < NR:
            stageD(step - 3)
    self.dump("mixna_b%d" % b, mix[:, 4:8, :], (128, 4, TT), BF16, [("mixn", h, r_) for h in range(8) for r_ in range(32)])


K.s2_l1 = _s2_l1


def _layer1(self, b):
    self.s2_l1(b)
    if self.stop in ("l1four", "l1s2", "l1fa", "l1fb"):
        return
    self.s3(b, 1)
    if self.stop == "l1s3":
        return
    self.s4_l1(b)
    self.s5(b, 1)


K.layer1 = _layer1


def _core_inputs(inputs, b0, NB, hc):
    m = {}
    m["x"] = np.ascontiguousarray(inputs["x"][b0:b0 + NB])
    m["c"] = np.ascontiguousarray(inputs["c"][b0:b0 + NB])
    m["ctx"] = np.ascontiguousarray(inputs["ctx"][b0:b0 + NB])
    for k in W_SHAPES:
        m[k] = np.ascontiguousarray(np.asarray(inputs[k], dtype=np.float32))
    rp = np.asarray(inputs["o_rpb"], dtype=np.float32)[0]
    m["rpbf"] = np.ascontiguousarray(rp[:, ::-1, ::-1]).reshape(8, 15 * 31)
    m["routerT"] = np.ascontiguousarray(np.asarray(inputs["o_router"], dtype=np.float32)[0].T)
    m.update(hc)
    return m


def run(inputs, NB=2, ncores=8, stop=None, dumps=(), trace=False):
    kb = K(NB=NB, stop=stop, dumps=dumps)
    nc = kb.build()
    hc = host_consts()
    in_maps = [_core_inputs(inputs, i * NB, NB, hc) for i in range(ncores)]
    res = run_bass_kernel_spmd(nc, in_maps, core_ids=list(range(ncores)), trace=trace)
    return kb, res


def kernel(**inputs):
    inputs = {k: np.asarray(v) for k, v in inputs.items()}
    kb, res = run(inputs, NB=2, ncores=8)
    out = np.concatenate([np.asarray(r["out"]) for r in res.results], axis=0)
    return out.astype(np.float32)
```
